# Optimizing a Trainium2 kernel written in Bass

```python
import jax, jax.numpy as jnp
from jax import lax
import numpy as np

D_MODEL = 1024
BATCH = 2
SEQ = 8192
DEPTH = 1

MEM_LEN = 256
HEAD_DIM = 64
LRU_WIDTH = D_MODEL // 2
LRU_BLOCKS = 8
LRU_BLOCK_W = LRU_WIDTH // LRU_BLOCKS
CONV_W = 4
LRU_C = 8.0
MOBA_HEADS = 4
MOBA_WIDTH = MOBA_HEADS * HEAD_DIM
MOBA_BLOCK = 256
MOBA_TOPK = 3
Q_CHUNK = 128
MEM_HEADS = 4
MEM_WIDTH = MEM_HEADS * HEAD_DIM
MIX_WIDTH = LRU_WIDTH + MOBA_WIDTH + MEM_WIDTH
IN_WIDTH = 2 * LRU_WIDTH + 3 * MOBA_WIDTH + MEM_WIDTH
D_FF = ((8 * D_MODEL // 3 + 127) // 128) * 128
NORM_EPS = 1e-6

kernel_name = "hymba_rglru_moba_memxattn_macaron"


def rms_norm(x, g):
    xf = x.astype(jnp.float32)
    y = xf * lax.rsqrt(jnp.mean(xf * xf, axis=-1, keepdims=True) + NORM_EPS)
    return (y * g.astype(jnp.float32)).astype(x.dtype)


def swiglu(x, w_in, w_out):
    a, b = jnp.split(x @ w_in, 2, axis=-1)
    return (jax.nn.silu(a) * b) @ w_out


def alibi_slopes(n_heads):
    return 2.0 ** (-8.0 * jnp.arange(1, n_heads + 1, dtype=jnp.float32) / n_heads)


def split_heads(t, n_heads):
    b, s, _ = t.shape
    return t.reshape(b, s, n_heads, HEAD_DIM).transpose(0, 2, 1, 3)


def merge_heads(t):
    b, h, s, d = t.shape
    return t.transpose(0, 2, 1, 3).reshape(b, s, h * d)


def causal_depthwise_conv(x, w, bias):
    c = x.shape[-1]
    y = lax.conv_general_dilated(
        x, w[:, None, :].astype(x.dtype), window_strides=(1,), padding=[(CONV_W - 1, 0)],
        dimension_numbers=("NWC", "WIO", "NWC"), feature_group_count=c)
    return y + bias.astype(x.dtype)


def rg_lru(xb, a_w, a_b, x_w, x_b, lam):
    b, s, c = xb.shape
    xf = xb.astype(jnp.float32)
    xblk = xf.reshape(b, s, LRU_BLOCKS, LRU_BLOCK_W)
    r = jax.nn.sigmoid(jnp.einsum("bsnc,ncd->bsnd", xblk, a_w.astype(jnp.float32)) + a_b.astype(jnp.float32)).reshape(b, s, c)
    i = jax.nn.sigmoid(jnp.einsum("bsnc,ncd->bsnd", xblk, x_w.astype(jnp.float32)) + x_b.astype(jnp.float32)).reshape(b, s, c)
    log_a = -LRU_C * r * jax.nn.softplus(-lam.astype(jnp.float32))
    a = jnp.exp(log_a)
    mult = jnp.sqrt(-jnp.expm1(2.0 * log_a))
    mult = jnp.where(jnp.arange(s)[None, :, None] == 0, 1.0, mult)
    bt = mult * i * xf

    def combine(left, right):
        a1, b1 = left
        a2, b2 = right
        return a1 * a2, a2 * b1 + b2

    _, h = lax.associative_scan(combine, (a, bt), axis=1)
    return h


def moba_attention(q, k, v):
    b, h, s, hd = q.shape
    n_blk = -(-s // MOBA_BLOCK)
    s_pad = n_blk * MOBA_BLOCK
    pad = s_pad - s
    if pad:
        widths = ((0, 0), (0, 0), (0, pad), (0, 0))
        q, k, v = jnp.pad(q, widths), jnp.pad(k, widths), jnp.pad(v, widths)
    kb = k.reshape(b, h, n_blk, MOBA_BLOCK, hd)
    vb = v.reshape(b, h, n_blk, MOBA_BLOCK, hd)
    k_mean = jnp.mean(kb.astype(jnp.float32), axis=3)
    gate = jnp.einsum("bhsd,bhnd->bhsn", q.astype(jnp.float32), k_mean)
    q_blk = jnp.arange(s_pad) // MOBA_BLOCK
    fully_past = jnp.arange(n_blk)[None, :] < q_blk[:, None]
    gate = jnp.where(fully_past, gate, -jnp.inf)
    n_sel = min(MOBA_TOPK, n_blk)
    gate_top, sel = lax.top_k(gate, n_sel)
    sel_ok = gate_top > -jnp.inf
    slopes = alibi_slopes(h)
    scale = hd ** -0.5
    offs = jnp.arange(MOBA_BLOCK)
    gather_blocks = jax.vmap(jax.vmap(lambda blocks, idx: blocks[idx]))

    def chunk(c):
        start = c * Q_CHUNK
        qc = lax.dynamic_slice_in_dim(q, start, Q_CHUNK, axis=2)
        selc = lax.dynamic_slice_in_dim(sel, start, Q_CHUNK, axis=2)
        okc = lax.dynamic_slice_in_dim(sel_ok, start, Q_CHUNK, axis=2)
        own = start // MOBA_BLOCK
        k_own = lax.dynamic_index_in_dim(kb, own, axis=2, keepdims=False)
        v_own = lax.dynamic_index_in_dim(vb, own, axis=2, keepdims=False)
        k_sel = gather_blocks(kb, selc)
        v_sel = gather_blocks(vb, selc)
        t_q = start + jnp.arange(Q_CHUNK)
        s_own = jnp.einsum("bhqd,bhkd->bhqk", qc, k_own).astype(jnp.float32) * scale
        d_own = t_q[:, None] - (own * MOBA_BLOCK + offs)[None, :]
        s_own = jnp.where(d_own >= 0, s_own - slopes[:, None, None] * d_own.astype(jnp.float32), -jnp.inf)
        s_sel = jnp.einsum("bhqd,bhqjkd->bhqjk", qc, k_sel).astype(jnp.float32) * scale
        d_sel = t_q[None, None, :, None, None] - (selc[..., None] * MOBA_BLOCK + offs)
        s_sel = jnp.where(okc[..., None], s_sel - slopes[None, :, None, None, None] * d_sel.astype(jnp.float32), -jnp.inf)
        scores = jnp.concatenate([s_sel.reshape(b, h, Q_CHUNK, n_sel * MOBA_BLOCK), s_own], axis=-1)
        p = jax.nn.softmax(scores, axis=-1)
        p_sel = p[..., :n_sel * MOBA_BLOCK].reshape(b, h, Q_CHUNK, n_sel, MOBA_BLOCK).astype(v.dtype)
        p_own = p[..., n_sel * MOBA_BLOCK:].astype(v.dtype)
        return (jnp.einsum("bhqjk,bhqjkd->bhqd", p_sel, v_sel)
                + jnp.einsum("bhqk,bhkd->bhqd", p_own, v_own))

    out = lax.map(chunk, jnp.arange(s_pad // Q_CHUNK))
    out = out.transpose(1, 2, 0, 3, 4).reshape(b, h, s_pad, hd)
    return out[:, :, :s]


def memory_attention(q, k, v):
    scores = jnp.einsum("bhsd,bhmd->bhsm", q, k).astype(jnp.float32) * (HEAD_DIM ** -0.5)
    p = jax.nn.softmax(scores, axis=-1)
    return jnp.einsum("bhsm,bhmd->bhsd", p.astype(v.dtype), v)


def setup_inputs(seed: int = 0) -> dict:
    key = jax.random.key(seed)
    ks = jax.random.split(key, 24)

    def normal(k, shape, scale):
        return jax.random.normal(k, shape, jnp.float32) * scale

    def gain(k, n):
        return 1.0 + 0.02 * jax.random.normal(k, (DEPTH, n), jnp.float32)

    u = jax.random.uniform(ks[12], (DEPTH, LRU_WIDTH), jnp.float32, minval=0.9, maxval=0.999)
    a0 = u ** (1.0 / LRU_C)
    lam = jnp.log(a0) - jnp.log1p(-a0)
    return {
        "x": normal(ks[0], (BATCH, SEQ, D_MODEL), 1.0),
        "mem": normal(ks[1], (BATCH, MEM_LEN, D_MODEL), 1.0),
        "ffn1_norm": gain(ks[2], D_MODEL),
        "ffn1_w_in": normal(ks[3], (DEPTH, D_MODEL, 2 * D_FF), D_MODEL ** -0.5),
        "ffn1_w_out": normal(ks[4], (DEPTH, D_FF, D_MODEL), D_FF ** -0.5),
        "mix_norm": gain(ks[5], D_MODEL),
        "mem_norm": gain(ks[6], D_MODEL),
        "w_in": normal(ks[7], (DEPTH, D_MODEL, IN_WIDTH), D_MODEL ** -0.5),
        "lru_conv_w": normal(ks[8], (DEPTH, CONV_W, LRU_WIDTH), CONV_W ** -0.5),
        "lru_conv_b": normal(ks[9], (DEPTH, LRU_WIDTH), 0.01),
        "lru_a_w": normal(ks[10], (DEPTH, LRU_BLOCKS, LRU_BLOCK_W, LRU_BLOCK_W), LRU_BLOCK_W ** -0.5),
        "lru_a_b": normal(ks[11], (DEPTH, LRU_BLOCKS, LRU_BLOCK_W), 0.01),
        "lru_x_w": normal(ks[13], (DEPTH, LRU_BLOCKS, LRU_BLOCK_W, LRU_BLOCK_W), LRU_BLOCK_W ** -0.5),
        "lru_x_b": normal(ks[14], (DEPTH, LRU_BLOCKS, LRU_BLOCK_W), 0.01),
        "lru_lambda": lam,
        "moba_q_norm": gain(ks[15], HEAD_DIM),
        "moba_k_norm": gain(ks[16], HEAD_DIM),
        "mem_w_kv": normal(ks[17], (DEPTH, D_MODEL, 2 * MEM_WIDTH), D_MODEL ** -0.5),
        "mem_q_norm": gain(ks[18], HEAD_DIM),
        "mem_k_norm": gain(ks[19], HEAD_DIM),
        "w_out": normal(ks[20], (DEPTH, MIX_WIDTH, D_MODEL), MIX_WIDTH ** -0.5),
        "ffn2_norm": gain(ks[21], D_MODEL),
        "ffn2_w_in": normal(ks[22], (DEPTH, D_MODEL, 2 * D_FF), D_MODEL ** -0.5),
        "ffn2_w_out": normal(ks[23], (DEPTH, D_FF, D_MODEL), D_FF ** -0.5),
    }


def reference(x, mem, ffn1_norm, ffn1_w_in, ffn1_w_out, mix_norm, mem_norm, w_in,
              lru_conv_w, lru_conv_b, lru_a_w, lru_a_b, lru_x_w, lru_x_b, lru_lambda,
              moba_q_norm, moba_k_norm, mem_w_kv, mem_q_norm, mem_k_norm, w_out,
              ffn2_norm, ffn2_w_in, ffn2_w_out):
    split_at = np.cumsum([LRU_WIDTH, LRU_WIDTH, MOBA_WIDTH, MOBA_WIDTH, MOBA_WIDTH])
    for l in range(DEPTH):
        x = x + 0.5 * swiglu(rms_norm(x, ffn1_norm[l]), ffn1_w_in[l], ffn1_w_out[l])

        u = rms_norm(x, mix_norm[l]) @ w_in[l]
        lru_x, lru_g, mq, mk, mv, cq = jnp.split(u, split_at, axis=-1)

        xb = causal_depthwise_conv(lru_x, lru_conv_w[l], lru_conv_b[l])
        h_lru = rg_lru(xb, lru_a_w[l], lru_a_b[l], lru_x_w[l], lru_x_b[l], lru_lambda[l])
        y_lru = (h_lru * jax.nn.gelu(lru_g.astype(jnp.float32))).astype(x.dtype)

        q = rms_norm(split_heads(mq, MOBA_HEADS), moba_q_norm[l])
        k = rms_norm(split_heads(mk, MOBA_HEADS), moba_k_norm[l])
        v = split_heads(mv, MOBA_HEADS)
        y_moba = merge_heads(moba_attention(q, k, v))

        mkv = rms_norm(mem, mem_norm[l]) @ mem_w_kv[l]
        mem_k, mem_v = jnp.split(mkv, 2, axis=-1)
        qc = rms_norm(split_heads(cq, MEM_HEADS), mem_q_norm[l])
        kc = rms_norm(split_heads(mem_k, MEM_HEADS), mem_k_norm[l])
        y_mem = merge_heads(memory_attention(qc, kc, split_heads(mem_v, MEM_HEADS)))

        mixed = jnp.concatenate([y_lru, y_moba.astype(x.dtype), y_mem.astype(x.dtype)], axis=-1)
        x = x + mixed @ w_out[l]

        x = x + 0.5 * swiglu(rms_norm(x, ffn2_norm[l]), ffn2_w_in[l], ffn2_w_out[l])
    return x
```

```python
import numpy as np
import ml_dtypes
from contextlib import ExitStack
import concourse.bass as bass
import concourse.mybir as mybir
from concourse.bass_utils import run_bass_kernel_spmd

F32 = mybir.dt.float32
BF16 = mybir.dt.bfloat16
ALU = mybir.AluOpType
AF = mybir.ActivationFunctionType
AX = mybir.AxisListType

D = 1024
DFF = 2816
NFC = 22
TOK = 2048
NT = 4
NB = 8
NBLK = 32
G = 4
EPS = 1e-6
NEG = -32768.0
SLOPES = [2.0 ** (-8.0 * (h + 1) / 4) for h in range(4)]


def gblock(j, i):
    return 4 * i + j if i % 2 == 0 else 4 * i + 3 - j


class Rec:
    ENGS = ("pe", "act", "dve", "pool", "sp")

    def __init__(self):
        self.ops = []
        self.slot_rr = {}

    def op(self, eng, fn, r=(), w=(), dma=None, inc=16, fence=True):
        w = list(w)
        if dma is not None:
            w.append(("dmaslot", dma))
        self.ops.append(dict(eng=eng, fn=fn, r=list(r), w=w, dma=dma, inc=inc, barrier=False, fence=fence))

    def dma(self, eng, fn, r=(), w=(), slots=("d", 4), fence=True):
        name, n = slots
        k = self.slot_rr.get(name, 0)
        self.slot_rr[name] = k + 1
        self.op(eng, fn, r, w, dma=(name, k % n), fence=fence)

    def barrier(self):
        for e in self.ENGS:
            self.ops.append(dict(eng=e, fn=None, r=[], w=[], dma=None, inc=0, barrier=True))

    def chan(self, k):
        o = self.ops[k]
        return ("dma",) + o["dma"] if o["dma"] is not None else o["eng"]

    def schedule(self):
        ops = self.ops
        n = len(ops)
        last_w, readers = {}, {}
        deps = [set() for _ in range(n)]
        last_real = {}
        dma_since = []
        for k, o in enumerate(ops):
            if o["barrier"]:
                deps[k] = set(last_real.values()) | set(dma_since)
                continue
            for x in o["r"]:
                if x in last_w:
                    deps[k].add(last_w[x])
                readers.setdefault(x, []).append(k)
            for x in o["w"]:
                if x in last_w:
                    deps[k].add(last_w[x])
                for rr in readers.get(x, []):
                    if rr != k:
                        deps[k].add(rr)
                last_w[x] = k
                readers[x] = []
            deps[k].discard(k)
            if o["fn"] is not None:
                if o["dma"] is None:
                    last_real[o["eng"]] = k
                elif o.get("fence", True):
                    dma_since.append(k)
                    if len(dma_since) > 64:
                        dma_since = dma_since[-64:]
        waited = {e: {} for e in self.ENGS}
        waits = [[] for _ in range(n)]
        marked = [False] * n
        for k, o in enumerate(ops):
            e = o["eng"]
            best = {}
            for d in deps[k]:
                c = self.chan(d)
                if c == "pe" and e == "pe":
                    continue
                if d > best.get(c, -1):
                    best[c] = d
            for c, d in best.items():
                if waited[e].get(c, -1) >= d:
                    continue
                waited[e][c] = d
                waits[k].append((c, d))
                marked[d] = True
        cnt = {}
        val = [0] * n
        for k, o in enumerate(ops):
            if o["fn"] is None:
                continue
            if o["dma"] is not None:
                marked[k] = True
            if marked[k]:
                c = self.chan(k)
                cnt[c] = cnt.get(c, 0) + (o["inc"] if o["dma"] is not None else 1)
                val[k] = cnt[c]
        self.waits, self.marked, self.val = waits, marked, val
        return sorted(set(self.chan(k) for k in range(n) if marked[k]), key=str)

    def emit(self, nc, block, sems):
        me = self

        def run(eng_name):
            def body(e):
                for k, o in enumerate(me.ops):
                    if o["eng"] != eng_name:
                        continue
                    for (c, d) in me.waits[k]:
                        e.wait_ge(sems[c], me.val[d])
                    if o["fn"] is None:
                        continue
                    ins = o["fn"](e)
                    if me.marked[k]:
                        c = me.chan(k)
                        if o["dma"] is not None:
                            ins.then_inc(sems[c], o["inc"])
                        else:
                            ins.then_inc(sems[c], 1)
            return body
        block.tensor(run("pe"))
        block.scalar(run("act"))
        block.vector(run("dve"))
        block.gpsimd(run("pool"))
        block.sync(run("sp"))


def _fm(v, nk):
    return np.ascontiguousarray(v.reshape(nk, 128).T)


def _host_tables():
    ident = np.eye(128, dtype=np.float32)
    blockones = np.zeros((128, 128), np.float32)
    blockones[:64, :64] = 1
    blockones[64:, 64:] = 1
    sel_even = np.zeros((128, 128), np.float32)
    sel_even[64, 0:64] = 1
    sel_odd = np.zeros((128, 128), np.float32)
    sel_odd[63, 64:128] = 1
    tk = np.arange(NBLK * 256)
    onehot = (tk[None, :] // 256 == np.arange(32)[:, None]).astype(np.float32)
    alik = np.stack([np.ones_like(tk), np.ones_like(tk), tk % 256, tk // 256]).astype(np.float32)
    kA = np.zeros((128, NBLK * 256), np.float32)
    kA[64:96] = onehot
    kA[96:100] = alik
    kB = np.zeros((128, NBLK * 256), np.float32)
    kB[0:32] = onehot
    kB[32:36] = alik
    kk = (np.arange(4)[None, :, None] * 128 + np.arange(128)[:, None, None])
    qq = np.arange(512)[None, None, :]
    ok = (kk // 256 == qq // 256) & (kk <= qq)
    ownmask = np.where(ok, 0.0, NEG).astype(np.float32)
    return dict(ident=ident, blockones=blockones, sel_even=sel_even, sel_odd=sel_odd,
                kaugA=kA.astype(ml_dtypes.bfloat16), kaugB=kB.astype(ml_dtypes.bfloat16),
                ownmask=ownmask.astype(ml_dtypes.bfloat16))


def _core_tables(j):
    nb = [gblock(j, i) for i in range(NB)]
    tq = np.concatenate([n * 256 + np.arange(256) for n in nb])
    qal = np.zeros((4, 4, TOK), np.float32)
    for h in range(4):
        s = SLOPES[h]
        qal[h, 0] = -s * (tq % 256)
        qal[h, 1] = -s * 256 * (tq // 256)
        qal[h, 2] = s
        qal[h, 3] = s * 256
    kal = np.stack([np.ones(TOK), np.ones(TOK), tq % 256, tq // 256]).astype(np.float32)
    pastbias = np.zeros((NB, NBLK), np.float32)
    selprev = np.zeros((NB, NBLK), np.float32)
    for i, n in enumerate(nb):
        pastbias[i, n:] = -1e9
        if n > 0:
            selprev[i, n - 1] = 1.0
    first = np.full((128, 1), 1.0 if j == 0 else 0.0, np.float32)
    return dict(qal=qal.astype(ml_dtypes.bfloat16), kal=kal.astype(ml_dtypes.bfloat16),
                pastbias=np.ascontiguousarray(np.broadcast_to(pastbias.reshape(1, -1), (128, NB * NBLK))),
                selprev=np.ascontiguousarray(np.broadcast_to(selprev.reshape(1, -1), (128, NB * NBLK))),
                first=first)


def _prep_weights(inp):
    o = {}

    def ffn(pref, w_in, w_out):
        wi = w_in.reshape(8, 128, 2, NFC, 128)
        o[pref + "_in"] = np.ascontiguousarray(wi.transpose(3, 1, 0, 2, 4)).reshape(NFC * 128, 2048)
        wo = w_out.reshape(NFC, 128, 8, 128)
        o[pref + "_out"] = np.ascontiguousarray(wo.transpose(2, 1, 0, 3)).reshape(8 * 128, NFC * 128)
    ffn("w1", inp["ffn1_w_in"][0], inp["ffn1_w_out"][0])
    ffn("w2", inp["ffn2_w_in"][0], inp["ffn2_w_out"][0])
    w = inp["w_in"][0]
    cols = np.concatenate([np.arange(0, 1536), np.arange(1792, 2048)])
    wf = w[:, cols].reshape(8, 128, 14, 128)
    o["wp"] = np.ascontiguousarray(wf.transpose(2, 1, 0, 3)).reshape(14 * 128, 1024)
    wv = w[:, 1536:1792].reshape(8, 128, 256)
    o["wpv"] = np.ascontiguousarray(wv.transpose(1, 0, 2)).reshape(128, 2048)
    wo = inp["w_out"][0].reshape(8, 128, 8, 128)
    o["wo"] = np.ascontiguousarray(wo.transpose(2, 1, 0, 3)).reshape(8 * 128, 1024)
    wm = inp["mem_w_kv"][0]
    wmk = wm[:, :256].reshape(8, 128, 2, 128)
    o["wmk"] = np.ascontiguousarray(wmk.transpose(2, 1, 0, 3)).reshape(2 * 128, 1024)
    wmv = wm[:, 256:].reshape(8, 128, 256)
    o["wmv"] = np.ascontiguousarray(wmv.transpose(1, 0, 2)).reshape(128, 2048)
    sm = {}
    sm["g1"] = _fm(inp["ffn1_norm"][0], 8)
    sm["gm"] = _fm(inp["mix_norm"][0], 8)
    sm["gmem"] = _fm(inp["mem_norm"][0], 8)
    sm["g2"] = _fm(inp["ffn2_norm"][0], 8)
    cw = inp["lru_conv_w"][0]
    sm["cw"] = np.ascontiguousarray(cw.reshape(4, 4, 128).transpose(2, 1, 0)).reshape(128, 16)
    sm["cb"] = _fm(inp["lru_conv_b"][0], 4)
    sm["ab"] = _fm(inp["lru_a_b"][0].reshape(-1), 4)
    sm["xb"] = _fm(inp["lru_x_b"][0].reshape(-1), 4)
    sm["lam"] = _fm(inp["lru_lambda"][0], 4)
    for nm, key in (("gq", "moba_q_norm"), ("gk", "moba_k_norm"), ("gcq", "mem_q_norm"), ("gck", "mem_k_norm")):
        sm[nm] = np.tile(inp[key][0], 2).reshape(128, 1)
    offs, cur, parts = {}, 0, []
    for k, v in sm.items():
        v = np.asarray(v, np.float32)
        offs[k] = (cur, v.shape[1])
        cur += v.shape[1]
        parts.append(v)
    o["small"] = np.ascontiguousarray(np.concatenate(parts, axis=1))
    bd = np.zeros((128, 2, 4, 128), np.float32)
    for t, key in enumerate(("lru_a_w", "lru_x_w")):
        wg = inp[key][0]
        for n in range(8):
            c, l = n // 2, n % 2
            bd[l * 64:(l + 1) * 64, t, c, l * 64:(l + 1) * 64] = wg[n]
    o["bd"] = bd.reshape(128, 1024)
    return o, offs


SM_OFFS = None


def build(sm_offs, ns, debug=False, stop=99):
    nc = bass.Bass("TRN2", target_bir_lowering=False)
    R = Rec()
    es = ExitStack()

    def MM(out, lhsT, rhs, start, stop, r, w):
        R.op("pe", lambda e: e.matmul(out, lhsT=lhsT, rhs=rhs, start=start, stop=stop), r, w)

    def ACT(out, in_, func, r, w, **kw):
        R.op("act", lambda e: e.activation(out=out, in_=in_, func=func, **kw), r, w)

    def TS(eng, out, in0, s1, s2, op0, op1, r, w):
        if op1 is None:
            R.op(eng, lambda e: e.tensor_scalar(out=out, in0=in0, scalar1=s1, scalar2=s2, op0=op0), r, w)
        else:
            R.op(eng, lambda e: e.tensor_scalar(out=out, in0=in0, scalar1=s1, scalar2=s2, op0=op0, op1=op1), r, w)

    def TT(eng, out, in0, in1, op, r, w):
        R.op(eng, lambda e: e.tensor_tensor(out=out, in0=in0, in1=in1, op=op), r, w)

    def STT(eng, out, in0, scalar, in1, op0, op1, r, w):
        R.op(eng, lambda e: e.scalar_tensor_tensor(out=out, in0=in0, scalar=scalar, in1=in1, op0=op0, op1=op1), r, w)

    def CP(eng, out, in_, r, w):
        R.op(eng, lambda e: e.tensor_copy(out=out, in_=in_), r, w)

    def RED(eng, out, in_, r, w):
        R.op(eng, lambda e: e.tensor_reduce(out=out, in_=in_, axis=AX.X, op=ALU.add), r, w)

    def SCAN(out, d0, d1, init, r, w):
        R.op("dve", lambda e: e.tensor_tensor_scan(out=out, data0=d0, data1=d1, initial=init, op0=ALU.mult, op1=ALU.add), r, w)

    def MSET(eng, ap, v, w):
        R.op(eng, lambda e: e.memset(ap, v), [], w)

    def DMA(eng, out, in_, r, w, slots, fence=True):
        R.dma(eng, lambda e: e.dma_start(out=out, in_=in_), r, w, slots, fence)

    def AG(src, dst, r, w, k):
        R.op("pool", lambda e: e.collective_compute("AllGather", ALU.bypass, replica_groups=[[0, 1, 2, 3], [4, 5, 6, 7]],
                                                    ins=[src.ap().opt()], outs=[dst.ap().opt()]),
             r, w, dma=("cc", k), inc=1)

    def din(name, shape, dt=F32):
        return nc.dram_tensor(name, list(shape), dt, kind="ExternalInput").ap()

    def dscr(name, shape, dt):
        return nc.dram_tensor(name, list(shape), dt)

    xT_d = din("xT", [128, 8, TOK])
    memT_d = din("memT", [128, 8, 256])
    w1i_d = din("w1_in", [NFC * 128, 2048]); w1o_d = din("w1_out", [1024, DFF])
    w2i_d = din("w2_in", [NFC * 128, 2048]); w2o_d = din("w2_out", [1024, DFF])
    wp_d = din("wp", [14 * 128, 1024]); wpv_d = din("wpv", [128, 2048])
    wo_d = din("wo", [1024, 1024]); wmk_d = din("wmk", [256, 1024]); wmv_d = din("wmv", [128, 2048])
    small_d = din("small", [128, ns]); bd_d = din("bd", [128, 1024])
    ident_d = din("ident", [128, 128]); bones_d = din("blockones", [128, 128])
    sele_d = din("sel_even", [128, 128]); selo_d = din("sel_odd", [128, 128])
    kaugA_d = din("kaugA", [128, 8192], BF16); kaugB_d = din("kaugB", [128, 8192], BF16)
    ownmask_d = din("ownmask", [128, 4, 512], BF16)
    qal_d = din("qal", [4, 4, TOK], BF16); kal_d = din("kal", [4, TOK], BF16)
    pastb_d = din("pastbias", [128, NB * NBLK]); selp_d = din("selprev", [128, NB * NBLK])
    first_d = din("first", [128, 1])
    out_d = nc.dram_tensor("outT", [128, 8, TOK], F32, kind="ExternalOutput").ap()
    dbg = {}
    if debug:
        for nm, shp in (("d_ylru", [128, 4, TOK]), ("d_ymoba", [128, 2, TOK]), ("d_ymem", [128, 2, TOK]),
                        ("d_lx", [128, 4, NB, 259]), ("d_q", [128, 4, TOK]), ("d_k", [128, 4, TOK]),
                        ("d_x1", [128, 8, TOK]), ("d_x2", [128, 8, TOK])):
            dbg[nm] = nc.dram_tensor(nm, shp, F32, kind="ExternalOutput").ap()

    w1i_s = dscr("w1i_s", [NFC * 128, 2048], BF16); w1o_s = dscr("w1o_s", [1024, DFF], BF16)
    w2i_s = dscr("w2i_s", [NFC * 128, 2048], BF16); w2o_s = dscr("w2o_s", [1024, DFF], BF16)
    x1_s = dscr("x1_s", [128, 8, TOK], F32)
    x2_s = dscr("x2_s", [128, 8, TOK], F32)
    s1_in = dscr("s1_in", [128, 112], F32); s1_out = dscr("s1_out", [G * 128, 112], F32)
    s2_in = dscr("s2_in", [128, 64], F32); s2_out = dscr("s2_out", [G * 128, 64], F32)
    k_in = dscr("k_in", [256, TOK], BF16); k_out = dscr("k_out", [G * 256, TOK], BF16)
    v_in = dscr("v_in", [TOK, 256], BF16); v_out = dscr("v_out", [G * TOK, 256], BF16)

    def sb(name, shape, dt, stack=None):
        return (stack or es).enter_context(nc.sbuf_tensor("sb_" + name, list(shape), dt))

    PS = es.enter_context(nc.psum_tensor("PS", [128, 8, 512], F32))
    small = sb("small", [128, ns], F32)
    gsc = sb("gsc", [128, 40], F32)
    ident_f = sb("ident_f", [128, 128], F32)
    ident = sb("ident", [128, 128], BF16)
    ones_b = sb("ones_b", [128, 128], BF16)
    bones_f = sb("bones_f", [128, 128], F32)
    bones = sb("bones", [128, 128], BF16)
    sel_e = sb("sel_e", [128, 128], F32)
    sel_o = sb("sel_o", [128, 128], F32)

    def S(k):
        o, n = sm_offs[k]
        return small[:, o:o + n]

    def bank(k, n=1):
        return PS[:, k, :] if n == 1 else PS[:, k:k + n, :]

    ALLX = [("xT", t, kc) for t in range(NT) for kc in range(8)]

    def XT(t):
        return [("xT", t, kc) for kc in range(8)]

    def XK(kc):
        return [("xT", t, kc) for t in range(NT)]

    LD = ("ld", 8)
    DMA("sp", small[:], small_d[:, :], [], ["small"], LD)
    DMA("sp", ident_f[:], ident_d[:, :], [], ["ident_f"], LD)
    DMA("sp", bones_f[:], bones_d[:, :], [], ["bones_f"], LD)
    DMA("sp", sel_e[:], sele_d[:, :], [], ["sel_e"], LD)
    DMA("sp", sel_o[:], selo_d[:, :], [], ["sel_o"], LD)
    CP("dve", ident[:], ident_f[:], ["ident_f"], ["ident"])
    CP("dve", bones[:], bones_f[:], ["bones_f"], ["bones"])
    MSET("dve", ones_b[:], 1.0, ["ones_b"])
    for q, nm in enumerate(("g1", "gm", "gmem", "g2")):
        TS("dve", gsc[:, 8 * q:8 * q + 8], S(nm), 32.0, None, ALU.mult, None, ["small"], ["gsc"])
    for q, (nm, f) in enumerate((("gq", 1.0), ("gk", 8.0), ("gcq", 1.0), ("gck", 8.0))):
        TS("dve", gsc[:, 32 + q:33 + q], S(nm), f, None, ALU.mult, None, ["small"], ["gsc"])
    ACT(gsc[:, 36:40], S("lam"), AF.Exp, ["small"], ["gsc"], scale=-1.0)
    ACT(gsc[:, 36:40], gsc[:, 36:40], AF.Ln, ["gsc"], ["gsc"], bias=1.0)
    TS("dve", gsc[:, 36:40], gsc[:, 36:40], -8.0, None, ALU.mult, None, ["gsc"], ["gsc"])
    R.barrier()

    def cast_rows(dst, src, nrows, tag, after=()):
        for g0 in range(0, nrows, 128):
            DMA("pool", dst[g0:g0 + 128, :], src[g0:g0 + 128, :], list(after), [(tag, g0 // 128)], ("cast", 4), fence=False)
    wp_s = dscr("wp_s", [14 * 128, 1024], BF16); wpv_s = dscr("wpv_s", [128, 2048], BF16)
    wo_s2 = dscr("wo_s2", [1024, 1024], BF16); wmk_s = dscr("wmk_s", [256, 1024], BF16)
    wmv_s = dscr("wmv_s", [128, 2048], BF16); bd_s = dscr("bd_s", [128, 1024], BF16)

    def norm_tile(src3, src_kc, gcol, xn_t, xnb, sq, rstd, tagx):
        ACT(sq[:], src3, AF.Square, tagx, ["sq"])
        for kc in range(8):
            MM(bank(6), ones_b[:], sq[:, kc, :], kc == 0, kc == 7, ["sq", "ones_b"], [("ps", 6)])
        ACT(rstd[:], bank(6), AF.Ln, [("ps", 6)], ["rstd"], bias=1024.0 * EPS)
        ACT(rstd[:], rstd[:], AF.Exp, ["rstd"], ["rstd"], scale=-0.5)
        for kc in range(8):
            STT("dve", xn_t[:, kc, :], src_kc(kc), gsc[:, gcol + kc:gcol + kc + 1], rstd[:], ALU.mult, ALU.mult,
                [tagx[kc], "rstd"], [(xnb, kc)])
        return [(xnb, kc) for kc in range(8)]

    def ffn(xT, wi_s, wo_s, gcol, tagw, stack, prefetch=None, post_tile=None, direct=None):
        xn = [sb(f"xn{b}{tagw}", [128, 8, 512], BF16, stack) for b in range(2)]
        sq = sb("sq" + tagw, [128, 8, 512], BF16, stack)
        rstd = sb("rstd" + tagw, [128, 512], F32, stack)
        gT = sb("gT" + tagw, [128, NFC, 512], BF16, stack)
        sa = [sb(f"sa{b}{tagw}", [128, 512], F32, stack) for b in range(2)]
        wib = [sb(f"wib{b}{tagw}", [128, 2, 2048], BF16, stack) for b in range(3)]
        wob = [sb(f"wob{b}{tagw}", [128, 2, DFF], BF16, stack) for b in range(2)]
        gi = go = fcn = yn = 0
        for t in range(NT):
            cols = slice(t * 512, (t + 1) * 512)
            xnk = norm_tile(xT[:, :, cols], lambda kc, cols=cols: xT[:, kc, cols], gcol, xn[t % 2], "xn%d" % (t % 2), sq, rstd, XT(t))
            for g in range(NFC // 2):
                b = gi % 3
                gi += 1
                if direct is not None and t == 0:
                    for f_ in range(2):
                        r0 = (2 * g + f_) * 128
                        DMA("pool", wib[b][:, f_, :], direct[r0:r0 + 128, :], [], [("wib", b, f_)], ("castd", 4))
                        DMA("sp", wi_s[r0:r0 + 128, :], wib[b][:, f_, :], [("wib", b, f_)], [(tagw + "i", 2 * g + f_)], ("wst", 4))
                else:
                    DMA("sp", wib[b][:], wi_s[g * 256:(g + 1) * 256, :].rearrange("(f p) x -> p f x", p=128),
                        [(tagw + "i", 2 * g), (tagw + "i", 2 * g + 1)], [("wib", b, 0), ("wib", b, 1)], ("wi", 3))
                if g == 2 and prefetch is not None and t + 1 < NT:
                    prefetch(t + 1)
                for f in range(2):
                    fc = 2 * g + f
                    pb = (fcn % 2) * 2
                    k = fcn % 2
                    for ab in range(2):
                        for kc in range(8):
                            o0 = kc * 256 + ab * 128
                            MM(bank(pb + ab), wib[b][:, f, o0:o0 + 128], xn[t % 2][:, kc, :], kc == 0, kc == 7,
                               [("wib", b, f), xnk[kc]], [("ps", pb + ab)])
                    ACT(sa[k][:], bank(pb), AF.Silu, [("ps", pb)], [("sa", k)])
                    TT("dve", gT[:, fc, :], sa[k][:], bank(pb + 1), ALU.mult, [("sa", k), ("ps", pb + 1)], [("gT", fc)])
                    fcn += 1
            for g in range(4):
                b = go % 2
                go += 1
                DMA("sp", wob[b][:], wo_s[g * 256:(g + 1) * 256, :].rearrange("(f p) x -> p f x", p=128),
                    [(tagw + "o", 2 * g), (tagw + "o", 2 * g + 1)], [("wob", b)], ("wo", 2))
                for f in range(2):
                    dc = 2 * g + f
                    yb = 4 + yn % 2
                    yn += 1
                    for fc in range(NFC):
                        MM(bank(yb), wob[b][:, f, fc * 128:(fc + 1) * 128], gT[:, fc, :], fc == 0, fc == NFC - 1,
                           [("wob", b), ("gT", fc)], [("ps", yb)])
                    STT("dve", xT[:, dc, cols], bank(yb), 0.5, xT[:, dc, cols], ALU.mult, ALU.add, [("ps", yb)], [("xT", t, dc)])
            if post_tile is not None:
                post_tile(t)

    with ExitStack() as st:
        xT = sb("xT1", [128, 8, TOK], F32, st)
        def xload(t):
            for kc in range(8):
                DMA("sp", xT[:, kc, t * 512:(t + 1) * 512], xT_d[:, kc, t * 512:(t + 1) * 512], [], [("xT", t, kc)], ("xin", 8))
            if t == 1:
                cast_rows(w1o_s, w1o_d, 1024, "w1o")
            if t == 2:
                for (d_, s_, n_, tg_) in ((wp_s, wp_d, 14 * 128, "wps"), (wpv_s, wpv_d, 128, "wpvs"), (wmk_s, wmk_d, 256, "wmks"),
                                          (wmv_s, wmv_d, 128, "wmvs"), (bd_s, bd_d, 128, "bds"), (wo_s2, wo_d, 1024, "wos")):
                    cast_rows(d_, s_, n_, tg_)
        xload(0)
        def spill1(t):
            for kc in range(8):
                DMA("sp", x1_s[:, kc, t * 512:(t + 1) * 512], xT[:, kc, t * 512:(t + 1) * 512], [("xT", t, kc)], [("x1_s", t, kc)], ("sp1", 8))
        ffn(xT, w1i_s, w1o_s, 0, "w1", st, prefetch=xload, post_tile=spill1, direct=w1i_d)
        if debug:
            DMA("sp", dbg["d_x1"][:, :, :], xT[:], ALLX, [], ("dbg", 2))
        if stop == 1:
            for kc in range(8):
                DMA("sp", out_d[:, kc, :], xT[:, kc, :], XK(kc), [("out", kc)], ("st", 8))
            R.ops.append(dict(eng="sp", fn=None, r=[("out", kc) for kc in range(8)], w=[], dma=None, inc=0, barrier=False))
        R.barrier()

    def rest():
        X1S = [("x1_s", kc) for kc in range(8)]

        with ExitStack() as st:
            QA = [sb(f"QA{h}", [128, TOK], BF16, st) for h in range(4)]
            KO = [sb(f"KO{h}", [128, TOK], BF16, st) for h in range(4)]
            VO = sb("VO", [128, 16, 260], BF16, st)
            CQ = sb("CQ", [128, 2, TOK], BF16, st)
            GG = sb("GG", [128, 4, TOK], BF16, st)
            KM = sb("KM", [128, 2, NB], F32, st)
            KMH = sb("KMH", [128, 2, NBLK], BF16, st)
            KML = sb("KML", [128, 2, NBLK], BF16, st)
            MK = sb("MK", [128, 2, 256], BF16, st)
            MV = sb("MV", [128, 2, 260], BF16, st)
            pastb = sb("pastb", [128, NB, NBLK], F32, st)
            selp = sb("selp", [128, NB, NBLK], F32, st)
            first = sb("first", [128, 1], F32, st)
            stLX = st
            LX = sb("LX", [128, 4, NB, 259], F32, stLX)
            S1 = sb("S1", [128, 112], F32, stLX)

            def rows(h):
                return slice(0, 64) if h % 2 == 0 else slice(64, 128)

            def vcols(h):
                return slice(65 * h, 65 * h + 128) if h % 2 == 0 else slice(65 * h - 64, 65 * h + 64)

            DMA("sp", pastb[:].rearrange("p a b -> p (a b)"), pastb_d[:, :], [], ["pastb"], LD)
            DMA("sp", selp[:].rearrange("p a b -> p (a b)"), selp_d[:, :], [], ["selp"], LD)
            DMA("sp", first[:], first_d[:, :], [], ["first"], LD)
            MSET("pool", VO[:], 1.0, [("VO", q) for q in range(16)])
            MSET("pool", MV[:], 1.0, ["MV"])
            for h in range(4):
                MSET("pool", QA[h][:], 0.0, [("QA", h)])
                MSET("pool", KO[h][:], 0.0, [("KO", h)])
                ar = slice(96, 100) if h % 2 == 0 else slice(32, 36)
                DMA("sp", QA[h][ar, :], qal_d[h, :, :], [], [("QA", h)], LD)
                DMA("sp", KO[h][ar, :], kal_d[:, :], [], [("KO", h)], LD)

            with ExitStack() as st2:
                WP = sb("WP", [128, 14, 1024], BF16, st2)
                WPV = sb("WPV", [128, 8, 256], BF16, st2)
                sq = sb("psq", [128, 8, 512], BF16, st2)
                rstd = sb("prstd", [128, 512], F32, st2)
                tb = sb("tb0", [128, 512], BF16, st2)
                hr = sb("hr", [128, 512], F32, st2)
                for cc in range(14):
                    DMA("sp", WP[:, cc, :], wp_s[cc * 128:(cc + 1) * 128, :], [("wps", cc)], [("WP", cc)], ("wl", 4))
                DMA("sp", WPV[:].rearrange("p a b -> p (a b)"), wpv_s[:, :], [("wpvs", 0)], ["WPV"], ("wl", 4))

                hcnt = [0]

                def headnorm(ps_ap, n, gc, outs, tag_r, tags_w):
                    hb = 6 + hcnt[0] % 2
                    hcnt[0] += 1
                    ACT(tb[:, :n], ps_ap, AF.Square, [tag_r], ["tb"])
                    MM(PS[:, hb, :n], bones[:], tb[:, :n], True, True, ["tb", "bones"], [("ps", hb)])
                    ACT(hr[:, :n], PS[:, hb, :n], AF.Ln, [("ps", hb)], ["hr"], bias=64.0 * EPS)
                    ACT(hr[:, :n], hr[:, :n], AF.Exp, ["hr"], ["hr"], scale=-0.5)
                    for (dst, rs_), tw in zip(outs, tags_w):
                        STT("dve", dst, ps_ap[rs_, :], gsc[rs_, gc:gc + 1], hr[rs_, :n], ALU.mult, ALU.mult, [tag_r, "hr"], [tw])

                with ExitStack() as stm:
                    WMK = sb("WMK", [128, 2, 1024], BF16, stm)
                    WMV = sb("WMV", [128, 8, 256], BF16, stm)
                    memT = sb("memT", [128, 8, 256], F32, stm)
                    memn = sb("memn", [128, 8, 256], BF16, stm)
                    for cc in range(2):
                        DMA("sp", WMK[:, cc, :], wmk_s[cc * 128:(cc + 1) * 128, :], [("wmks", cc)], ["WMK"], ("wl", 4))
                    DMA("sp", WMV[:].rearrange("p a b -> p (a b)"), wmv_s[:, :], [("wmvs", 0)], ["WMV"], ("wl", 4))
                    DMA("sp", memT[:], memT_d[:, :, :], [], ["memT"], LD)
                    ACT(sq[:, :, :256], memT[:], AF.Square, ["memT"], ["sq"])
                    for kc in range(8):
                        MM(PS[:, 6, :256], ones_b[:], sq[:, kc, :256], kc == 0, kc == 7, ["sq", "ones_b"], [("ps", 6)])
                    ACT(rstd[:, :256], PS[:, 6, :256], AF.Ln, [("ps", 6)], ["rstd"], bias=1024.0 * EPS)
                    ACT(rstd[:, :256], rstd[:, :256], AF.Exp, ["rstd"], ["rstd"], scale=-0.5)
                    for kc in range(8):
                        STT("dve", memn[:, kc, :], memT[:, kc, :], gsc[:, 16 + kc:17 + kc], rstd[:, :256], ALU.mult, ALU.mult,
                            ["memT", "rstd"], [("memn", kc)])
                    for cc in range(2):
                        for kc in range(8):
                            MM(PS[:, cc, :256], WMK[:, cc, kc * 128:(kc + 1) * 128], memn[:, kc, :], kc == 0, kc == 7,
                               ["WMK", ("memn", kc)], [("ps", cc)])
                        headnorm(PS[:, cc, :256], 256, 35, [(MK[0:64, cc, :], slice(0, 64)), (MK[64:128, cc, :], slice(64, 128))],
                                 ("ps", cc), [("MK", cc), ("MK", cc)])
                    for mt in range(2):
                        for kc in range(8):
                            MM(PS[:, 2 + mt, :256], memn[:, kc, mt * 128:(mt + 1) * 128], WMV[:, kc, :], kc == 0, kc == 7,
                               ["WMV", ("memn", kc)], [("ps", 2 + mt)])
                        ACT(MV[:, mt, :].rearrange("p (h e) -> p h e", e=65)[:, :, 0:64],
                            PS[:, 2 + mt, :256].rearrange("p (h e) -> p h e", e=64), AF.Copy, [("ps", 2 + mt)], ["MV"])
                    R.barrier()

                xn = [sb(f"pxn{b}", [128, 8, 512], BF16, st2) for b in range(4)]
                tf = [sb(f"tf{b}", [128, 512], F32, st2) for b in range(4)]
                xtt = sb("xt0", [128, 8, 512], F32, st2)
                LXA = [("LX", c) for c in range(4)]
                uc = [0]
                xnks = []

                def proj(t, cc):
                    cols = slice(t * 512, (t + 1) * 512)
                    xnk = xnks[t]
                    ub = uc[0] % 4
                    uc[0] += 1
                    for kc in range(8):
                        MM(bank(ub), WP[:, cc, kc * 128:(kc + 1) * 128], xn[t][:, kc, :], kc == 0, kc == 7,
                           [("WP", cc), xnk[kc]], [("ps", ub)])
                    pt = ("ps", ub)
                    if cc < 4:
                        ACT(LX[:, cc, 2 * t:2 * t + 2, 3:259], bank(ub).rearrange("p (a b) -> p a b", b=256), AF.Copy, [pt], [("LX", cc)])
                    elif cc < 8:
                        c = cc - 4
                        k = uc[0] % 2
                        g0, g1 = tf[2 * k], tf[2 * k + 1]
                        t0, t1_ = ("tf", 2 * k), ("tf", 2 * k + 1)
                        ACT(g0[:], bank(ub), AF.Copy, [pt], [t0])
                        TT("pool", g1[:], g0[:], g0[:], ALU.mult, [t0], [t1_])
                        TS("pool", g1[:], g1[:], 0.044715, 1.0, ALU.mult, ALU.add, [t1_], [t1_])
                        TT("pool", g1[:], g1[:], g0[:], ALU.mult, [t0, t1_], [t1_])
                        ACT(g1[:], g1[:], AF.Sigmoid, [t1_], [t1_], scale=1.5957691216057308)
                        TT("dve", GG[:, c, cols], g0[:], g1[:], ALU.mult, [t0, t1_], [("GG", c)])
                    elif cc < 10:
                        p = cc - 8
                        headnorm(bank(ub), 512, 32, [(QA[2 * p][0:64, cols], slice(0, 64)), (QA[2 * p + 1][64:128, cols], slice(64, 128))],
                                 pt, [("QA", 2 * p), ("QA", 2 * p + 1)])
                    elif cc < 12:
                        p = cc - 10
                        headnorm(bank(ub), 512, 33, [(tf[0][0:64, :], slice(0, 64)), (tf[0][64:128, :], slice(64, 128))],
                                 pt, [("tf", 0), ("tf", 0)])
                        RED("dve", KM[:, p, 2 * t:2 * t + 2], tf[0][:].rearrange("p (a b) -> p a b", b=256), [("tf", 0)], [("KM", p)])
                        ACT(KO[2 * p][0:64, cols], tf[0][0:64, :], AF.Copy, [("tf", 0)], [("KO", 2 * p)])
                        ACT(KO[2 * p + 1][64:128, cols], tf[0][64:128, :], AF.Copy, [("tf", 0)], [("KO", 2 * p + 1)])
                    else:
                        p = cc - 12
                        headnorm(bank(ub), 512, 34, [(CQ[0:64, p, cols], slice(0, 64)), (CQ[64:128, p, cols], slice(64, 128))],
                                 pt, [("CQ", p), ("CQ", p)])

                for t in range(NT):
                    cols = slice(t * 512, (t + 1) * 512)
                    for kc in range(8):
                        DMA("sp", xtt[:, kc, :], x1_s[:, kc, cols], [("x1_s", t, kc)], [("xt", kc)], ("xl", 8))
                    xnks.append(norm_tile(xtt[:], lambda kc: xtt[:, kc, :], 8, xn[t], "xn%d" % t, sq, rstd,
                                          [("xt", kc) for kc in range(8)]))
                    for cc in (10, 11, 0, 1, 2, 3):
                        proj(t, cc)
                TS("dve", KM[:], KM[:], 1.0 / 256.0, None, ALU.mult, None, [("KM", 0), ("KM", 1)], [("KM", 0), ("KM", 1)])
                CP("pool", S1[:, 0:96].rearrange("p (c i k) -> p c i k", c=4, i=NB), LX[:, :, :, 256:259], LXA, ["S1"])
                CP("pool", S1[:, 96:112], KM[:].rearrange("p a b -> p (a b)"), [("KM", 0), ("KM", 1)], ["S1"])
                DMA("pool", s1_in[:, :], S1[:], ["S1"], ["s1_in"], ("x1", 2))
                AG(s1_in, s1_out, ["s1_in"], ["s1_out"], 0)
                for h in range(4):
                    DMA("pool", k_in[h * 64:(h + 1) * 64, :], KO[h][rows(h), :], [("KO", h)], [("k_in", h)], ("x2", 4))
                AG(k_in, k_out, [("k_in", h) for h in range(4)], ["k_out"], 1)
                for t in range(NT):
                    xnk = xnks[t]
                    for s_ in range(4):
                        vb = 4 + s_ % 2
                        for kc in range(8):
                            MM(PS[:, vb, :256], xn[t][:, kc, s_ * 128:(s_ + 1) * 128], WPV[:, kc, :], kc == 0, kc == 7,
                               ["WPV", xnk[kc]], [("ps", vb)])
                        ACT(VO[:, 4 * t + s_, :].rearrange("p (h e) -> p h e", e=65)[:, :, 0:64],
                            PS[:, vb, :256].rearrange("p (h e) -> p h e", e=64), AF.Copy, [("ps", vb)], [("VO", 4 * t + s_)])
                for q in range(16):
                    DMA("pool", v_in[q * 128:(q + 1) * 128, :].rearrange("p (h e) -> p h e", e=64),
                        VO[:, q, :].rearrange("p (h e) -> p h e", e=65)[:, :, 0:64], [("VO", q)], [("v_in", q)], ("x2", 4))
                AG(v_in, v_out, [("v_in", q) for q in range(16)], ["v_out"], 2)
                for t in range(NT):
                    for cc in (8, 9, 12, 13, 4, 5, 6, 7):
                        proj(t, cc)
                R.barrier()
            if stop == 2:
                return

            if stop == 3:
                R.barrier()
                return

            def compute_masks(gm16, t8, thr, mpad):
                for h in range(4):
                    p = h % 2
                    mrow = slice(64, 96) if h % 2 == 0 else slice(0, 32)
                    for qt in range(16):
                        qs = slice(qt * 128, (qt + 1) * 128)
                        MM(PS[:, 7, qt * 32:(qt + 1) * 32], QA[h][rows(h), qs], KMH[rows(h), h // 2, :], True, False, [("QA", h), "KMH"], [("ps", 7)])
                        MM(PS[:, 7, qt * 32:(qt + 1) * 32], QA[h][rows(h), qs], KML[rows(h), h // 2, :], False, True, [("QA", h), "KML"], [("ps", 7)])
                    TT("dve", gm16[:].rearrange("p (i t) n -> p i t n", t=2), PS[:, 7, :].rearrange("p (i t n) -> p i t n", t=2, n=NBLK),
                       pastb[:].unsqueeze(2).broadcast_to([128, NB, 2, NBLK]), ALU.add, [("ps", 7), "pastb"], ["gm16"])
                    for qt in range(16):
                        R.op("dve", (lambda qt: lambda e: e.max(out=t8[:, qt, :], in_=gm16[:, qt, :]))(qt), ["gm16"], [("t8", qt)])
                    TS("dve", thr[:], t8[:, :, 2], -1e8, None, ALU.max, None, [("t8", qt) for qt in range(16)], ["thr"])
                    TT("dve", gm16[:], gm16[:], thr[:].unsqueeze(2).broadcast_to([128, 16, NBLK]), ALU.is_ge, ["gm16", "thr"], ["gm16"])
                    TS("dve", mpad[p][:, :, mrow], gm16[:], -1.0, None, ALU.add, None, ["gm16"], [("mpad", p)])
                    for qt in range(16):
                        MM(PS[:, qt // 4, (qt % 4) * 128:(qt % 4 + 1) * 128], mpad[p][:, qt, :], ident[:], True, True,
                           [("mpad", p), "ident"], [("ps", qt // 4)])
                    ACT(QA[h][mrow, :].rearrange("m (b w) -> m b w", w=512), PS[mrow, 0:4, :], AF.Copy,
                        [("ps", b) for b in range(4)], [("QA", h)], scale=32768.0)


            def run_blocks(blocks, PT, Osbs, rdens, load_k=None, obanks=(6,)):
                NSB = len(PT)
                oc = [0]
                def stageA(n):
                    bk = blocks[n]
                    if bk["pre"] is not None:
                        load_k(bk["pre"])
                    b = n % NSB
                    for s in range(2):
                        MM(bank(2 * b + s), bk["lhs"][s], bk["rhs"], True, bk["extra"] is None, bk["rt"], [("ps", 2 * b + s)])
                        if bk["extra"] is not None:
                            MM(bank(2 * b + s), ident[:], bk["extra"][s], False, True, ["ident", "om"], [("ps", 2 * b + s)])
                    ACT(PT[b][:], bank(2 * b, 2), AF.Exp, [("ps", 2 * b), ("ps", 2 * b + 1)], [("PT", b)])

                def stageB(n):
                    bk = blocks[n]
                    b = n % NSB
                    ob = obanks[oc[0] % len(obanks)]
                    k2 = oc[0] % len(Osbs)
                    Osb, rden = Osbs[k2], rdens[k2]
                    for s in range(2):
                        MM(bank(ob), bk["v"][s], PT[b][:, s, :], bk["first"] and s == 0, bk["last"] and s == 1, [("PT", b)] + bk["rt"], [("ps", ob)])
                    if bk["tail"] is not None:
                        oc[0] += 1
                        h, ydst, tagy = bk["tail"]
                        CP("dve", Osb[:], bank(ob), [("ps", ob)], [("Osb", k2)])
                        selm, tg = (sel_e, "sel_e") if h % 2 == 0 else (sel_o, "sel_o")
                        MM(bank(7), selm[:], Osb[:], True, True, [("Osb", k2), tg], [("ps", 7)])
                        R.op("dve", lambda e: e.reciprocal(out=rden[rows(h), :], in_=PS[rows(h), 7, :]), [("ps", 7)], [("rden", k2)])
                        TT("dve", ydst, Osb[rows(h), :], rden[rows(h), :], ALU.mult, [("Osb", k2), ("rden", k2)],
                           [tagy] + (["attn_started"] if bk.get("mark") else []))

                NBK = len(blocks)
                LEAD = NSB - 1
                for n in range(NBK + LEAD):
                    if n < NBK:
                        stageA(n)
                    if n >= LEAD:
                        stageB(n - LEAD)

            YC = sb("YC", [128, 2, TOK], BF16, st)
            with ExitStack() as st3:
                G1 = sb("G1", [128, G, 112], F32, st3)
                HG = sb("HG", [128, 4, NBLK, 3], F32, st3)
                KMG = sb("KMG", [128, 2, NBLK], F32, st3)
                BD = sb("BD", [128, 2, 4, 128], BF16, st3)
                BT = sb("BT", [128, 4, TOK], F32, st3)
                xb = sb("xb", [128, TOK], F32, st3)
                xbb = sb("xbb", [128, TOK], BF16, st3)
                rr = sb("rr", [128, TOK], F32, st3)
                ii = sb("ii", [128, TOK], F32, st3)
                a2 = sb("a2", [128, TOK], F32, st3)
                hl = sb("hl", [128, TOK], F32, st3)
                tmpS = sb("tmpS", [128, 12, NBLK], F32, st3)
                C2 = sb("C2", [128, 4, NB, 2], F32, st3)
                G2 = sb("G2", [128, G, 64], F32, st3)
                CG = sb("CG", [128, 4, NBLK, 2], F32, st3)
                HS = sb("HS", [128, 4, NBLK], F32, st3)
                hin = sb("hin", [128, 4, NB], F32, st3)
                rs = sb("rs", [128, NB], F32, st3)
                t1 = sb("t1", [128, 4], F32, st3)

                DMA("sp", BD[:].rearrange("p a b c -> p (a b c)"), bd_s[:, :], [("bds", 0)], ["BD"], ("wl", 4))
                DMA("sp", G1[:], s1_out[:, :].rearrange("(r p) x -> p r x", p=128), ["s1_out"], ["G1"], LD)
                for r_ in range(G):
                    for par in range(2):
                        n0 = r_ if par == 0 else 7 - r_
                        CP("pool", HG[:, :, n0::8, :], G1[:, r_, 0:96].rearrange("p (c i k) -> p c i k", c=4, i=NB)[:, :, par::2, :], ["G1"], ["HG"])
                        CP("pool", KMG[:, :, n0::8], G1[:, r_, 96:112].rearrange("p (a i) -> p a i", a=2)[:, :, par::2], ["G1"], ["KMG"])
                CP("dve", KMH[:], KMG[:], ["KMG"], ["KMH"])
                TT("dve", KMG[:], KMG[:], KMH[:], ALU.subtract, ["KMG", "KMH"], ["KMG"])
                CP("dve", KML[:], KMG[:], ["KMG"], ["KML"])
                for i in range(NB):
                    TT("dve", tmpS[:].rearrange("p (c k) n -> p c k n", c=4), HG[:].rearrange("p c n k -> p c k n"),
                       selp[:, i, :].unsqueeze(1).unsqueeze(1).broadcast_to([128, 4, 3, NBLK]), ALU.mult, ["HG", "selp"], ["tmpS"])
                    RED("dve", LX[:, :, i, 0:3], tmpS[:].rearrange("p (c k) n -> p c k n", c=4), ["tmpS"], LXA)
                if debug:
                    DMA("sp", dbg["d_lx"][:, :, :, :], LX[:], LXA, [], ("dbg", 2))

                cwo = sm_offs["cw"][0]
                xb3 = xb[:].rearrange("p (i w) -> p i w", w=256)
                for c in range(4):
                    lx = LX[:, c, :, :]
                    LXc = ("LX", c)

                    def cwc(k, c=c):
                        return small[:, cwo + 4 * c + k:cwo + 4 * c + k + 1]
                    TS("pool", xb3, lx[:, :, 3:259], cwc(3), S("cb")[:, c:c + 1], ALU.mult, ALU.add, [LXc], ["xb"])
                    for k in range(3):
                        STT("dve", xb3, lx[:, :, k:k + 256], cwc(k), xb3, ALU.mult, ALU.add, [LXc, "xb"], ["xb"])
                    ACT(xbb[:], xb[:], AF.Copy, ["xb"], ["xbb"])
                    for q in range(4):
                        MM(bank(q), BD[:, 0, c, :], xbb[:, q * 512:(q + 1) * 512], True, True, ["xbb", "BD"], [("ps", q)])
                        MM(bank(4 + q), BD[:, 1, c, :], xbb[:, q * 512:(q + 1) * 512], True, True, ["xbb", "BD"], [("ps", 4 + q)])
                    ACT(rr[:].rearrange("p (q w) -> p q w", w=512), bank(0, 4), AF.Sigmoid, [("ps", q) for q in range(4)], ["rr"],
                        bias=S("ab")[:, c:c + 1])
                    ACT(ii[:].rearrange("p (q w) -> p q w", w=512), bank(4, 4), AF.Sigmoid, [("ps", 4 + q) for q in range(4)], ["ii"],
                        bias=S("xb")[:, c:c + 1])
                    RED("dve", rs[:], rr[:].rearrange("p (i w) -> p i w", w=256), ["rr"], ["rs"])
                    ACT(C2[:, c, :, 0], rs[:], AF.Exp, ["rs"], [("C2", c)], scale=gsc[:, 36 + c:37 + c])
                    TS("dve", t1[:, c:c + 1], gsc[:, 36 + c:37 + c], 2.0, None, ALU.mult, None, [], [("t1", c)])
                    ACT(a2[:], rr[:], AF.Exp, ["rr", ("t1", c)], ["a2"], scale=t1[:, c:c + 1])
                    ACT(LX[:, c, :, 0:256], rr[:].rearrange("p (i w) -> p i w", w=256), AF.Exp, ["rr", "xb"], [LXc], scale=gsc[:, 36 + c:37 + c])
                    ACT(a2[:], a2[:], AF.Sqrt, ["a2"], ["a2"], scale=-1.0, bias=1.0)
                    TS("dve", rs[:, 0:1], a2[:, 0:1], -1.0, 1.0, ALU.mult, ALU.add, ["a2"], ["rs"])
                    STT("dve", a2[:, 0:1], rs[:, 0:1], first[:, 0:1], a2[:, 0:1], ALU.mult, ALU.add, ["rs", "first", "a2"], ["a2"])
                    TT("pool", ii[:], ii[:], xb[:], ALU.mult, ["ii", "xb"], ["ii"])
                    TT("dve", BT[:, c, :], ii[:], a2[:], ALU.mult, ["ii", "a2"], [("BT", c)])
                    for i in range(NB):
                        SCAN(hl[:, i * 256:(i + 1) * 256], LX[:, c, i, 0:256], BT[:, c, i * 256:(i + 1) * 256], 0.0, [LXc, ("BT", c)], [("hl", i)])
                    CP("dve", C2[:, c, :, 1], hl[:].rearrange("p (i w) -> p i w", w=256)[:, :, 255], [("hl", i) for i in range(NB)], [("C2", c)])
                R.barrier()
                DMA("pool", s2_in[:, :], C2[:].rearrange("p a b c -> p (a b c)"), [("C2", c) for c in range(4)], ["s2_in"], ("x1", 2))
                AG(s2_in, s2_out, ["s2_in"], ["s2_out"], 3)
                Osbm, rdenm = [xb[:, 0:512], xb[:, 512:1024]], [xb[:, 1024:1536], xb[:, 1536:2048]]
                gm16 = rr[:, 0:512].rearrange("p (a n) -> p a n", n=NBLK)
                t8 = rr[:, 512:640].rearrange("p (a k) -> p a k", k=8)
                thr = rr[:, 640:656]
                PTm = [xbb[:, 0:1024].rearrange("p (s w) -> p s w", w=512), xbb[:, 1024:2048].rearrange("p (s w) -> p s w", w=512)]
                hlb = hl[:].bitcast(BF16)
                mpad = [hlb[:, 0:2048].rearrange("p (a m) -> p a m", m=128), hlb[:, 2048:4096].rearrange("p (a m) -> p a m", m=128)]
                MSET("pool", mpad[0], 0.0, [("mpad", 0)])
                MSET("pool", mpad[1], 0.0, [("mpad", 1)])
                mblocks = []
                for h in range(4):
                    p = h // 2
                    for c in range(NT):
                        cols = slice(c * 512, (c + 1) * 512)
                        mblocks.append(dict(lhs=[MK[rows(h), p, 0:128], MK[rows(h), p, 128:256]], rhs=CQ[rows(h), p, cols],
                                            v=[MV[:, 0, vcols(h)], MV[:, 1, vcols(h)]], first=True, last=True,
                                            rt=[("MK", p), ("CQ", p), "MV"], extra=None, tail=(h, YC[rows(h), p, cols], ("YC", p)), pre=None))
                run_blocks(mblocks, PTm, Osbm, rdenm, obanks=(4, 5, 6))
                compute_masks(gm16, t8, thr, mpad)
                R.barrier()
                DMA("sp", G2[:], s2_out[:, :].rearrange("(r p) x -> p r x", p=128), ["s2_out"], ["G2"], LD)
                for r_ in range(G):
                    for par in range(2):
                        n0 = r_ if par == 0 else 7 - r_
                        CP("pool", CG[:, :, n0::8, :], G2[:, r_, :].rearrange("p (c i k) -> p c i k", c=4, i=NB)[:, :, par::2, :], ["G2"], ["CG"])
                for c in range(4):
                    SCAN(HS[:, c, :], CG[:, c, :, 0], CG[:, c, :, 1], 0.0, ["CG"], [("HS", c)])
                for i in range(NB):
                    TT("dve", tmpS[:, 0:4, :], HS[:], selp[:, i, :].unsqueeze(1).broadcast_to([128, 4, NBLK]), ALU.mult,
                       [("HS", c) for c in range(4)] + ["selp"], ["tmpS"])
                    RED("dve", hin[:, :, i], tmpS[:, 0:4, :], ["tmpS"], ["hin"])
                for c in range(4):
                    hb_, hbt = ((hl, "hl"), (rr, "rr"), (ii, "ii"), (a2, "a2"))[c]
                    for i in range(NB):
                        SCAN(hb_[:, i * 256:(i + 1) * 256], LX[:, c, i, 0:256], BT[:, c, i * 256:(i + 1) * 256], hin[:, c, i:i + 1],
                             [("LX", c), ("BT", c), "hin"], [(hbt + "2", i)])
                    TT("pool" if c % 2 == 0 else "dve", GG[:, c, :], hb_[:], GG[:, c, :], ALU.mult,
                       [(hbt + "2", i) for i in range(NB)] + [("GG", c)], [("GG", c)])
                if debug:
                    for c in range(4):
                        CP("dve", xb[:], GG[:, c, :], [("GG", c)], ["xb"])
                        DMA("sp", dbg["d_ylru"][:, c, :], xb[:], ["xb"], ["dbgo"], ("dbg", 2))
                R.barrier()

            YM = sb("YM", [128, 2, TOK], BF16, st)
            if stop == 4:
                return
            with ExitStack() as st4:
                KX = [sb(f"KX{p}", [128, NBLK * 256], BF16, st4) for p in range(2)]
                VA = sb("VA", [128, 64, 260], BF16, st4)
                om = sb("om", [128, 4, 512], BF16, st4)
                NSB = 3
                PT = [sb(f"PT{b}", [128, 2, 512], BF16, st4) for b in range(NSB)]
                Osb = sb("Osb", [128, 512], F32, st4)
                rden = sb("rden", [128, 512], F32, st4)

                MSET("pool", KX[0][:], 0.0, [("KX", 0, q) for q in range(8)])
                MSET("dve", KX[1][:], 0.0, [("KX", 1, q) for q in range(8)])
                DMA("sp", KX[0][64:100, :], kaugA_d[64:100, :], [], [("KX", 0, q) for q in range(8)], LD)
                DMA("sp", KX[1][0:36, :], kaugB_d[0:36, :], [], [("KX", 1, q) for q in range(8)], LD)
                DMA("sp", om[:], ownmask_d[:, :, :], [], ["om"], LD)
                MSET("pool", VA[:].rearrange("p t (h e) -> p t h e", e=65)[:, :, :, 64:65], 1.0, [("VA", q) for q in range(64)])
                for r_ in range(G):
                    for i in range(NB):
                        n = gblock(r_, i)
                        for s_ in range(2):
                            r0 = r_ * TOK + i * 256 + s_ * 128
                            DMA("sp", VA[:, 2 * n + s_, :].rearrange("p (h e) -> p h e", e=65)[:, :, 0:64],
                                v_out[r0:r0 + 128, :].rearrange("p (h e) -> p h e", e=64), ["v_out"], [("VA", 2 * n + s_)], ("vl", 8))

                def load_k(h):
                    p = h % 2
                    for r_ in range(G):
                        for par in range(2):
                            n0 = r_ if par == 0 else 7 - r_
                            DMA("sp", KX[p][rows(h), :].rearrange("q (c n w) -> q c n w", n=8, w=256)[:, :, n0, :],
                                k_out[r_ * 256 + h * 64:r_ * 256 + (h + 1) * 64, :].rearrange("q (c i w) -> q c i w", i=2, w=256)[:, :, par, :],
                                ["k_out"], [("KX", p, n0)], ("kl", 8))

                blocks = []
                for h in range(4):
                    p = h % 2
                    for c in range(NT):
                        cols = slice(c * 512, (c + 1) * 512)
                        nkb = 8 * c + 7
                        for kb in range(nkb):
                            blocks.append(dict(lhs=[KX[p][:, (2 * kb + s) * 128:(2 * kb + s + 1) * 128] for s in range(2)], rhs=QA[h][:, cols],
                                               v=[VA[:, 2 * kb + s, vcols(h)] for s in range(2)], first=(kb == 0), last=False,
                                               rt=[("KX", p, kb % 8), ("QA", h), ("VA", 2 * kb), ("VA", 2 * kb + 1)], extra=None, tail=None,
                                               pre=(h if (c == 0 and kb == 0) else None)))
                        for kb in range(2):
                            lt = [c * 512 + (2 * kb + s) * 128 for s in range(2)]
                            blocks.append(dict(lhs=[KO[h][:, l0:l0 + 128] for l0 in lt], rhs=QA[h][:, cols],
                                               v=[VO[:, 4 * c + 2 * kb + s, vcols(h)] for s in range(2)], first=False, last=(kb == 1),
                                               rt=[("KO", h), ("QA", h)] + [("VO", 4 * c + 2 * kb + s) for s in range(2)],
                                               extra=[om[:, 2 * kb + s, :] for s in range(2)],
                                               tail=((h, YM[rows(h), h // 2, cols], ("YM", h // 2)) if kb == 1 else None), pre=None,
                                               mark=(h == 0 and c == 0 and kb == 1)))

                run_blocks(blocks, PT, [Osb], [rden], load_k)
                cast_rows(w2i_s, w2i_d, NFC * 128, "w2i", after=["attn_started"])
                cast_rows(w2o_s, w2o_d, 1024, "w2o", after=["attn_started"])
                if debug:
                    dt8 = sb("dt8", [128, TOK], F32, st4)
                    for q in range(2):
                        CP("dve", dt8[:], YM[:, q, :], [("YM", q)], ["dt8"])
                        DMA("sp", dbg["d_ymoba"][:, q, :], dt8[:], ["dt8"], ["dbgo"], ("dbg", 2))
                    for q in range(2):
                        CP("dve", dt8[:], YC[:, q, :], [("YC", q)], ["dt8"])
                        DMA("sp", dbg["d_ymem"][:, q, :], dt8[:], ["dt8"], ["dbgo"], ("dbg", 2))
                    for h in range(4):
                        CP("dve", dt8[:], QA[h][:], [("QA", h)], ["dt8"])
                        DMA("sp", dbg["d_q"][:, h, :], dt8[:], ["dt8"], ["dbgo"], ("dbg", 2))
                    for h in range(4):
                        CP("dve", dt8[:], KO[h][:], [("KO", h)], ["dt8"])
                        DMA("sp", dbg["d_k"][:, h, :], dt8[:], ["dt8"], ["dbgo"], ("dbg", 2))
                R.barrier()

            if stop == 5:
                return
            with ExitStack() as st5:
                WO = sb("WO", [128, 8, 1024], BF16, st5)
                xt = [sb(f"yt{b}", [128, 8, 512], F32, st5) for b in range(2)]
                for dc in range(8):
                    DMA("sp", WO[:, dc, :], wo_s2[dc * 128:(dc + 1) * 128, :], [("wos", dc)], [("WO", dc)], ("wl", 4))
                yn = 0
                for t in range(NT):
                    cols = slice(t * 512, (t + 1) * 512)
                    xtt = xt[t % 2]
                    for kc in range(8):
                        DMA("sp", xtt[:, kc, :], x1_s[:, kc, cols], [("x1_s", t, kc)], [("yt", t % 2, kc)], ("xl", 4))
                    for dc in range(8):
                        yb = yn % 4
                        yn += 1
                        for fc in range(8):
                            if fc < 4:
                                rhs, tg = GG[:, fc, cols], ("GG", fc)
                            elif fc < 6:
                                rhs, tg = YM[:, fc - 4, cols], ("YM", fc - 4)
                            else:
                                rhs, tg = YC[:, fc - 6, cols], ("YC", fc - 6)
                            MM(bank(yb), WO[:, dc, fc * 128:(fc + 1) * 128], rhs, fc == 0, fc == 7, [("WO", dc), tg], [("ps", yb)])
                        TT("dve", xtt[:, dc, :], bank(yb), xtt[:, dc, :], ALU.add, [("ps", yb)], [("yt", t % 2, dc)])
                        DMA("sp", x2_s[:, dc, cols], xtt[:, dc, :], [("yt", t % 2, dc)], [("x2_s", dc)], ("xs", 4))
                R.barrier()

        with ExitStack() as st:
            xT = sb("xT2", [128, 8, TOK], F32, st)
            def x2load(t):
                for kc in range(8):
                    DMA("sp", xT[:, kc, t * 512:(t + 1) * 512], x2_s[:, kc, t * 512:(t + 1) * 512], [("x2_s", kc)], [("xT", t, kc)], ("xin", 8))
            x2load(0)
            if debug:
                DMA("sp", dbg["d_x2"][:, :, :], x2_s[:, :, :], [("x2_s", kc) for kc in range(8)], [], ("dbg", 2))
            ffn(xT, w2i_s, w2o_s, 24, "w2", st, prefetch=x2load)
            for kc in range(8):
                DMA("sp", out_d[:, kc, :], xT[:, kc, :], XK(kc), [("out", kc)], ("st", 8))
            R.ops.append(dict(eng="sp", fn=None, r=[("out", kc) for kc in range(8)], w=[], dma=None, inc=0, barrier=False))
            R.barrier()

    if stop > 1:
        rest()

    chans = R.schedule()
    sems = {c: es.enter_context(nc.semaphore("s_" + "_".join(str(x) for x in (c if isinstance(c, tuple) else (c,))))) for c in chans}
    block = es.enter_context(nc.Block())
    R.emit(nc, block, sems)
    es.close()
    return nc


_CACHE = {}


def kernel(**inp):
    debug = bool(inp.pop("_debug", False))
    inp = {k: np.asarray(v) for k, v in inp.items()}
    wts, offs = _prep_weights(inp)
    ns = wts["small"].shape[1]
    key = (debug,)
    if key not in _CACHE:
        _CACHE[key] = build(offs, ns, debug)
    nc = _CACHE[key]
    tabs = _host_tables()
    x, mem = inp["x"], inp["mem"]
    in_maps = []
    for core in range(8):
        b, j = core // 4, core % 4
        idx = np.concatenate([gblock(j, i) * 256 + np.arange(256) for i in range(NB)])
        xs = x[b][idx]
        xTc = np.ascontiguousarray(xs.T.reshape(8, 128, TOK).transpose(1, 0, 2))
        memT = np.ascontiguousarray(mem[b].T.reshape(8, 128, 256).transpose(1, 0, 2))
        m = dict(xT=xTc, memT=memT)
        m.update(wts)
        m.update(tabs)
        m.update(_core_tables(j))
        in_maps.append(m)
    res = run_bass_kernel_spmd(nc, in_maps, core_ids=list(range(8)))
    out = np.empty((2, 8192, D), np.float32)
    for core in range(8):
        b, j = core // 4, core % 4
        idx = np.concatenate([gblock(j, i) * 256 + np.arange(256) for i in range(NB)])
        oT = res.results[core]["outT"]
        out[b][idx] = oT.transpose(1, 0, 2).reshape(D, TOK).T
    if debug:
        kernel.last = res
    return out
```

```python
import numpy as np
import ml_dtypes
from contextlib import ExitStack
import concourse.bass as bass
import concourse.mybir as mybir
from concourse.bass_utils import run_bass_kernel_spmd

F32 = mybir.dt.float32
BF16 = mybir.dt.bfloat16
ALU = mybir.AluOpType
AF = mybir.ActivationFunctionType
AX = mybir.AxisListType

D = 1024
DFF = 2816
NFC = 22
TOK = 2048
NT = 4
NB = 8
NBLK = 32
G = 4
EPS = 1e-6
NEG = -32768.0
SLOPES = [2.0 ** (-8.0 * (h + 1) / 4) for h in range(4)]


def gblock(j, i):
    return 4 * i + j if i % 2 == 0 else 4 * i + 3 - j


class Rec:
    ENGS = ("pe", "act", "dve", "pool", "sp")

    def __init__(self):
        self.ops = []
        self.slot_rr = {}

    def op(self, eng, fn, r=(), w=(), dma=None, inc=16, fence=True):
        w = list(w)
        if dma is not None:
            w.append(("dmaslot", dma))
        self.ops.append(dict(eng=eng, fn=fn, r=list(r), w=w, dma=dma, inc=inc, barrier=False, fence=fence))

    def dma(self, eng, fn, r=(), w=(), slots=("d", 4), fence=True):
        name, n = slots
        k = self.slot_rr.get(name, 0)
        self.slot_rr[name] = k + 1
        self.op(eng, fn, r, w, dma=(name, k % n), fence=fence)

    def barrier(self):
        for e in self.ENGS:
            self.ops.append(dict(eng=e, fn=None, r=[], w=[], dma=None, inc=0, barrier=True))

    def chan(self, k):
        o = self.ops[k]
        return ("dma",) + o["dma"] if o["dma"] is not None else o["eng"]

    def schedule(self):
        ops = self.ops
        n = len(ops)
        last_w, readers = {}, {}
        deps = [set() for _ in range(n)]
        last_real = {}
        dma_since = []
        for k, o in enumerate(ops):
            if o["barrier"]:
                deps[k] = set(last_real.values()) | set(dma_since)
                continue
            for x in o["r"]:
                if x in last_w:
                    deps[k].add(last_w[x])
                readers.setdefault(x, []).append(k)
            for x in o["w"]:
                if x in last_w:
                    deps[k].add(last_w[x])
                for rr in readers.get(x, []):
                    if rr != k:
                        deps[k].add(rr)
                last_w[x] = k
                readers[x] = []
            deps[k].discard(k)
            if o["fn"] is not None:
                if o["dma"] is None:
                    last_real[o["eng"]] = k
                elif o.get("fence", True):
                    dma_since.append(k)
                    if len(dma_since) > 64:
                        dma_since = dma_since[-64:]
        waited = {e: {} for e in self.ENGS}
        waits = [[] for _ in range(n)]
        marked = [False] * n
        for k, o in enumerate(ops):
            e = o["eng"]
            best = {}
            for d in deps[k]:
                c = self.chan(d)
                if c == "pe" and e == "pe":
                    continue
                if d > best.get(c, -1):
                    best[c] = d
            for c, d in best.items():
                if waited[e].get(c, -1) >= d:
                    continue
                waited[e][c] = d
                waits[k].append((c, d))
                marked[d] = True
        cnt = {}
        val = [0] * n
        for k, o in enumerate(ops):
            if o["fn"] is None:
                continue
            if o["dma"] is not None:
                marked[k] = True
            if marked[k]:
                c = self.chan(k)
                cnt[c] = cnt.get(c, 0) + (o["inc"] if o["dma"] is not None else 1)
                val[k] = cnt[c]
        self.waits, self.marked, self.val = waits, marked, val
        return sorted(set(self.chan(k) for k in range(n) if marked[k]), key=str)

    def emit(self, nc, block, sems):
        me = self

        def run(eng_name):
            def body(e):
                for k, o in enumerate(me.ops):
                    if o["eng"] != eng_name:
                        continue
                    for (c, d) in me.waits[k]:
                        e.wait_ge(sems[c], me.val[d])
                    if o["fn"] is None:
                        continue
                    ins = o["fn"](e)
                    if me.marked[k]:
                        c = me.chan(k)
                        if o["dma"] is not None:
                            ins.then_inc(sems[c], o["inc"])
                        else:
                            ins.then_inc(sems[c], 1)
            return body
        block.tensor(run("pe"))
        block.scalar(run("act"))
        block.vector(run("dve"))
        block.gpsimd(run("pool"))
        block.sync(run("sp"))


def _fm(v, nk):
    return np.ascontiguousarray(v.reshape(nk, 128).T)


def _host_tables():
    ident = np.eye(128, dtype=np.float32)
    blockones = np.zeros((128, 128), np.float32)
    blockones[:64, :64] = 1
    blockones[64:, 64:] = 1
    sel_even = np.zeros((128, 128), np.float32)
    sel_even[64, 0:64] = 1
    sel_odd = np.zeros((128, 128), np.float32)
    sel_odd[63, 64:128] = 1
    tk = np.arange(NBLK * 256)
    onehot = (tk[None, :] // 256 == np.arange(32)[:, None]).astype(np.float32)
    alik = np.stack([np.ones_like(tk), np.ones_like(tk), tk % 256, tk // 256]).astype(np.float32)
    kA = np.zeros((128, NBLK * 256), np.float32)
    kA[64:96] = onehot
    kA[96:100] = alik
    kB = np.zeros((128, NBLK * 256), np.float32)
    kB[0:32] = onehot
    kB[32:36] = alik
    kk = (np.arange(4)[None, :, None] * 128 + np.arange(128)[:, None, None])
    qq = np.arange(512)[None, None, :]
    ok = (kk // 256 == qq // 256) & (kk <= qq)
    ownmask = np.where(ok, 0.0, NEG).astype(np.float32)
    return dict(ident=ident, blockones=blockones, sel_even=sel_even, sel_odd=sel_odd,
                kaugA=kA.astype(ml_dtypes.bfloat16), kaugB=kB.astype(ml_dtypes.bfloat16),
                ownmask=ownmask.astype(ml_dtypes.bfloat16))


def _core_tables(j):
    nb = [gblock(j, i) for i in range(NB)]
    tq = np.concatenate([n * 256 + np.arange(256) for n in nb])
    qal = np.zeros((4, 4, TOK), np.float32)
    for h in range(4):
        s = SLOPES[h]
        qal[h, 0] = -s * (tq % 256)
        qal[h, 1] = -s * 256 * (tq // 256)
        qal[h, 2] = s
        qal[h, 3] = s * 256
    kal = np.stack([np.ones(TOK), np.ones(TOK), tq % 256, tq // 256]).astype(np.float32)
    pastbias = np.zeros((NB, NBLK), np.float32)
    selprev = np.zeros((NB, NBLK), np.float32)
    for i, n in enumerate(nb):
        pastbias[i, n:] = -1e9
        if n > 0:
            selprev[i, n - 1] = 1.0
    first = np.full((128, 1), 1.0 if j == 0 else 0.0, np.float32)
    return dict(qal=qal.astype(ml_dtypes.bfloat16), kal=kal.astype(ml_dtypes.bfloat16),
                pastbias=np.ascontiguousarray(np.broadcast_to(pastbias.reshape(1, -1), (128, NB * NBLK))),
                selprev=np.ascontiguousarray(np.broadcast_to(selprev.reshape(1, -1), (128, NB * NBLK))),
                first=first)


def _prep_weights(inp):
    o = {}

    def ffn(pref, w_in, w_out):
        wi = w_in.reshape(8, 128, 2, NFC, 128)
        o[pref + "_in"] = np.ascontiguousarray(wi.transpose(3, 1, 0, 2, 4)).reshape(NFC * 128, 2048)
        wo = w_out.reshape(NFC, 128, 8, 128)
        o[pref + "_out"] = np.ascontiguousarray(wo.transpose(2, 1, 0, 3)).reshape(8 * 128, NFC * 128)
    ffn("w1", inp["ffn1_w_in"][0], inp["ffn1_w_out"][0])
    ffn("w2", inp["ffn2_w_in"][0], inp["ffn2_w_out"][0])
    w = inp["w_in"][0]
    cols = np.concatenate([np.arange(0, 1536), np.arange(1792, 2048)])
    wf = w[:, cols].reshape(8, 128, 14, 128)
    o["wp"] = np.ascontiguousarray(wf.transpose(2, 1, 0, 3)).reshape(14 * 128, 1024)
    wv = w[:, 1536:1792].reshape(8, 128, 256)
    o["wpv"] = np.ascontiguousarray(wv.transpose(1, 0, 2)).reshape(128, 2048)
    wo = inp["w_out"][0].reshape(8, 128, 8, 128)
    o["wo"] = np.ascontiguousarray(wo.transpose(2, 1, 0, 3)).reshape(8 * 128, 1024)
    wm = inp["mem_w_kv"][0]
    wmk = wm[:, :256].reshape(8, 128, 2, 128)
    o["wmk"] = np.ascontiguousarray(wmk.transpose(2, 1, 0, 3)).reshape(2 * 128, 1024)
    wmv = wm[:, 256:].reshape(8, 128, 256)
    o["wmv"] = np.ascontiguousarray(wmv.transpose(1, 0, 2)).reshape(128, 2048)
    sm = {}
    sm["g1"] = _fm(inp["ffn1_norm"][0], 8)
    sm["gm"] = _fm(inp["mix_norm"][0], 8)
    sm["gmem"] = _fm(inp["mem_norm"][0], 8)
    sm["g2"] = _fm(inp["ffn2_norm"][0], 8)
    cw = inp["lru_conv_w"][0]
    sm["cw"] = np.ascontiguousarray(cw.reshape(4, 4, 128).transpose(2, 1, 0)).reshape(128, 16)
    sm["cb"] = _fm(inp["lru_conv_b"][0], 4)
    sm["ab"] = _fm(inp["lru_a_b"][0].reshape(-1), 4)
    sm["xb"] = _fm(inp["lru_x_b"][0].reshape(-1), 4)
    sm["lam"] = _fm(inp["lru_lambda"][0], 4)
    for nm, key in (("gq", "moba_q_norm"), ("gk", "moba_k_norm"), ("gcq", "mem_q_norm"), ("gck", "mem_k_norm")):
        sm[nm] = np.tile(inp[key][0], 2).reshape(128, 1)
    offs, cur, parts = {}, 0, []
    for k, v in sm.items():
        v = np.asarray(v, np.float32)
        offs[k] = (cur, v.shape[1])
        cur += v.shape[1]
        parts.append(v)
    o["small"] = np.ascontiguousarray(np.concatenate(parts, axis=1))
    bd = np.zeros((128, 2, 4, 128), np.float32)
    for t, key in enumerate(("lru_a_w", "lru_x_w")):
        wg = inp[key][0]
        for n in range(8):
            c, l = n // 2, n % 2
            bd[l * 64:(l + 1) * 64, t, c, l * 64:(l + 1) * 64] = wg[n]
    o["bd"] = bd.reshape(128, 1024)
    return o, offs


SM_OFFS = None


def build(sm_offs, ns, debug=False, stop=99):
    nc = bass.Bass("TRN2", target_bir_lowering=False)
    R = Rec()
    es = ExitStack()

    def MM(out, lhsT, rhs, start, stop, r, w):
        R.op("pe", lambda e: e.matmul(out, lhsT=lhsT, rhs=rhs, start=start, stop=stop), r, w)

    def ACT(out, in_, func, r, w, **kw):
        R.op("act", lambda e: e.activation(out=out, in_=in_, func=func, **kw), r, w)

    def TS(eng, out, in0, s1, s2, op0, op1, r, w):
        if op1 is None:
            R.op(eng, lambda e: e.tensor_scalar(out=out, in0=in0, scalar1=s1, scalar2=s2, op0=op0), r, w)
        else:
            R.op(eng, lambda e: e.tensor_scalar(out=out, in0=in0, scalar1=s1, scalar2=s2, op0=op0, op1=op1), r, w)

    def TT(eng, out, in0, in1, op, r, w):
        R.op(eng, lambda e: e.tensor_tensor(out=out, in0=in0, in1=in1, op=op), r, w)

    def STT(eng, out, in0, scalar, in1, op0, op1, r, w):
        R.op(eng, lambda e: e.scalar_tensor_tensor(out=out, in0=in0, scalar=scalar, in1=in1, op0=op0, op1=op1), r, w)

    def CP(eng, out, in_, r, w):
        R.op(eng, lambda e: e.tensor_copy(out=out, in_=in_), r, w)

    def RED(eng, out, in_, r, w):
        R.op(eng, lambda e: e.tensor_reduce(out=out, in_=in_, axis=AX.X, op=ALU.add), r, w)

    def SCAN(out, d0, d1, init, r, w):
        R.op("dve", lambda e: e.tensor_tensor_scan(out=out, data0=d0, data1=d1, initial=init, op0=ALU.mult, op1=ALU.add), r, w)

    def MSET(eng, ap, v, w):
        R.op(eng, lambda e: e.memset(ap, v), [], w)

    def DMA(eng, out, in_, r, w, slots, fence=True):
        R.dma(eng, lambda e: e.dma_start(out=out, in_=in_), r, w, slots, fence)

    def AG(src, dst, r, w, k):
        R.op("pool", lambda e: e.collective_compute("AllGather", ALU.bypass, replica_groups=[[0, 1, 2, 3], [4, 5, 6, 7]],
                                                    ins=[src.ap().opt()], outs=[dst.ap().opt()]),
             r, w, dma=("cc", k), inc=1)

    def din(name, shape, dt=F32):
        return nc.dram_tensor(name, list(shape), dt, kind="ExternalInput").ap()

    def dscr(name, shape, dt):
        return nc.dram_tensor(name, list(shape), dt)

    xT_d = din("xT", [128, 8, TOK])
    memT_d = din("memT", [128, 8, 256])
    w1i_d = din("w1_in", [NFC * 128, 2048]); w1o_d = din("w1_out", [1024, DFF])
    w2i_d = din("w2_in", [NFC * 128, 2048]); w2o_d = din("w2_out", [1024, DFF])
    wp_d = din("wp", [14 * 128, 1024]); wpv_d = din("wpv", [128, 2048])
    wo_d = din("wo", [1024, 1024]); wmk_d = din("wmk", [256, 1024]); wmv_d = din("wmv", [128, 2048])
    small_d = din("small", [128, ns]); bd_d = din("bd", [128, 1024])
    ident_d = din("ident", [128, 128]); bones_d = din("blockones", [128, 128])
    sele_d = din("sel_even", [128, 128]); selo_d = din("sel_odd", [128, 128])
    kaugA_d = din("kaugA", [128, 8192], BF16); kaugB_d = din("kaugB", [128, 8192], BF16)
    ownmask_d = din("ownmask", [128, 4, 512], BF16)
    qal_d = din("qal", [4, 4, TOK], BF16); kal_d = din("kal", [4, TOK], BF16)
    pastb_d = din("pastbias", [128, NB * NBLK]); selp_d = din("selprev", [128, NB * NBLK])
    first_d = din("first", [128, 1])
    out_d = nc.dram_tensor("outT", [128, 8, TOK], F32, kind="ExternalOutput").ap()
    dbg = {}
    if debug:
        for nm, shp in (("d_ylru", [128, 4, TOK]), ("d_ymoba", [128, 2, TOK]), ("d_ymem", [128, 2, TOK]),
                        ("d_lx", [128, 4, NB, 259]), ("d_q", [128, 4, TOK]), ("d_k", [128, 4, TOK]),
                        ("d_x1", [128, 8, TOK]), ("d_x2", [128, 8, TOK])):
            dbg[nm] = nc.dram_tensor(nm, shp, F32, kind="ExternalOutput").ap()

    w1i_s = dscr("w1i_s", [NFC * 128, 2048], BF16); w1o_s = dscr("w1o_s", [1024, DFF], BF16)
    w2i_s = dscr("w2i_s", [NFC * 128, 2048], BF16); w2o_s = dscr("w2o_s", [1024, DFF], BF16)
    x1_s = dscr("x1_s", [128, 8, TOK], F32)
    x2_s = dscr("x2_s", [128, 8, TOK], F32)
    s1_in = dscr("s1_in", [128, 112], F32); s1_out = dscr("s1_out", [G * 128, 112], F32)
    s2_in = dscr("s2_in", [128, 64], F32); s2_out = dscr("s2_out", [G * 128, 64], F32)
    k_in = dscr("k_in", [256, TOK], BF16); k_out = dscr("k_out", [G * 256, TOK], BF16)
    v_in = dscr("v_in", [TOK, 256], BF16); v_out = dscr("v_out", [G * TOK, 256], BF16)

    def sb(name, shape, dt, stack=None):
        return (stack or es).enter_context(nc.sbuf_tensor("sb_" + name, list(shape), dt))

    PS = es.enter_context(nc.psum_tensor("PS", [128, 8, 512], F32))
    small = sb("small", [128, ns], F32)
    gsc = sb("gsc", [128, 40], F32)
    ident_f = sb("ident_f", [128, 128], F32)
    ident = sb("ident", [128, 128], BF16)
    ones_b = sb("ones_b", [128, 128], BF16)
    bones_f = sb("bones_f", [128, 128], F32)
    bones = sb("bones", [128, 128], BF16)
    sel_e = sb("sel_e", [128, 128], F32)
    sel_o = sb("sel_o", [128, 128], F32)

    def S(k):
        o, n = sm_offs[k]
        return small[:, o:o + n]

    def bank(k, n=1):
        return PS[:, k, :] if n == 1 else PS[:, k:k + n, :]

    ALLX = [("xT", t, kc) for t in range(NT) for kc in range(8)]

    def XT(t):
        return [("xT", t, kc) for kc in range(8)]

    def XK(kc):
        return [("xT", t, kc) for t in range(NT)]

    LD = ("ld", 8)
    DMA("sp", small[:], small_d[:, :], [], ["small"], LD)
    DMA("sp", ident_f[:], ident_d[:, :], [], ["ident_f"], LD)
    DMA("sp", bones_f[:], bones_d[:, :], [], ["bones_f"], LD)
    DMA("sp", sel_e[:], sele_d[:, :], [], ["sel_e"], LD)
    DMA("sp", sel_o[:], selo_d[:, :], [], ["sel_o"], LD)
    CP("dve", ident[:], ident_f[:], ["ident_f"], ["ident"])
    CP("dve", bones[:], bones_f[:], ["bones_f"], ["bones"])
    MSET("dve", ones_b[:], 1.0, ["ones_b"])
    for q, nm in enumerate(("g1", "gm", "gmem", "g2")):
        TS("dve", gsc[:, 8 * q:8 * q + 8], S(nm), 32.0, None, ALU.mult, None, ["small"], ["gsc"])
    for q, (nm, f) in enumerate((("gq", 1.0), ("gk", 8.0), ("gcq", 1.0), ("gck", 8.0))):
        TS("dve", gsc[:, 32 + q:33 + q], S(nm), f, None, ALU.mult, None, ["small"], ["gsc"])
    ACT(gsc[:, 36:40], S("lam"), AF.Exp, ["small"], ["gsc"], scale=-1.0)
    ACT(gsc[:, 36:40], gsc[:, 36:40], AF.Ln, ["gsc"], ["gsc"], bias=1.0)
    TS("dve", gsc[:, 36:40], gsc[:, 36:40], -8.0, None, ALU.mult, None, ["gsc"], ["gsc"])
    R.barrier()

    def cast_rows(dst, src, nrows, tag, after=()):
        for g0 in range(0, nrows, 128):
            DMA("pool", dst[g0:g0 + 128, :], src[g0:g0 + 128, :], list(after), [(tag, g0 // 128)], ("cast", 4), fence=False)
    wp_s = dscr("wp_s", [14 * 128, 1024], BF16); wpv_s = dscr("wpv_s", [128, 2048], BF16)
    wo_s2 = dscr("wo_s2", [1024, 1024], BF16); wmk_s = dscr("wmk_s", [256, 1024], BF16)
    wmv_s = dscr("wmv_s", [128, 2048], BF16); bd_s = dscr("bd_s", [128, 1024], BF16)

    def norm_tile(src3, src_kc, gcol, xn_t, xnb, sq, rstd, tagx):
        ACT(sq[:], src3, AF.Square, tagx, ["sq"])
        for kc in range(8):
            MM(bank(6), ones_b[:], sq[:, kc, :], kc == 0, kc == 7, ["sq", "ones_b"], [("ps", 6)])
        ACT(rstd[:], bank(6), AF.Ln, [("ps", 6)], ["rstd"], bias=1024.0 * EPS)
        ACT(rstd[:], rstd[:], AF.Exp, ["rstd"], ["rstd"], scale=-0.5)
        for kc in range(8):
            STT("dve", xn_t[:, kc, :], src_kc(kc), gsc[:, gcol + kc:gcol + kc + 1], rstd[:], ALU.mult, ALU.mult,
                [tagx[kc], "rstd"], [(xnb, kc)])
        return [(xnb, kc) for kc in range(8)]

    def ffn(xT, wi_s, wo_s, gcol, tagw, stack, prefetch=None, post_tile=None, direct=None):
        xn = [sb(f"xn{b}{tagw}", [128, 8, 512], BF16, stack) for b in range(2)]
        sq = sb("sq" + tagw, [128, 8, 512], BF16, stack)
        rstd = sb("rstd" + tagw, [128, 512], F32, stack)
        gT = sb("gT" + tagw, [128, NFC, 512], BF16, stack)
        sa = [sb(f"sa{b}{tagw}", [128, 512], F32, stack) for b in range(2)]
        wib = [sb(f"wib{b}{tagw}", [128, 2, 2048], BF16, stack) for b in range(3)]
        wob = [sb(f"wob{b}{tagw}", [128, 2, DFF], BF16, stack) for b in range(2)]
        gi = go = fcn = yn = 0
        for t in range(NT):
            cols = slice(t * 512, (t + 1) * 512)
            xnk = norm_tile(xT[:, :, cols], lambda kc, cols=cols: xT[:, kc, cols], gcol, xn[t % 2], "xn%d" % (t % 2), sq, rstd, XT(t))
            for g in range(NFC // 2):
                b = gi % 3
                gi += 1
                if direct is not None and t == 0:
                    for f_ in range(2):
                        r0 = (2 * g + f_) * 128
                        DMA("pool", wib[b][:, f_, :], direct[r0:r0 + 128, :], [], [("wib", b, f_)], ("castd", 4))
                        DMA("sp", wi_s[r0:r0 + 128, :], wib[b][:, f_, :], [("wib", b, f_)], [(tagw + "i", 2 * g + f_)], ("wst", 4))
                else:
                    DMA("sp", wib[b][:], wi_s[g * 256:(g + 1) * 256, :].rearrange("(f p) x -> p f x", p=128),
                        [(tagw + "i", 2 * g), (tagw + "i", 2 * g + 1)], [("wib", b, 0), ("wib", b, 1)], ("wi", 3))
                if g == 2 and prefetch is not None and t + 1 < NT:
                    prefetch(t + 1)
                for f in range(2):
                    fc = 2 * g + f
                    pb = (fcn % 2) * 2
                    k = fcn % 2
                    for ab in range(2):
                        for kc in range(8):
                            o0 = kc * 256 + ab * 128
                            MM(bank(pb + ab), wib[b][:, f, o0:o0 + 128], xn[t % 2][:, kc, :], kc == 0, kc == 7,
                               [("wib", b, f), xnk[kc]], [("ps", pb + ab)])
                    ACT(sa[k][:], bank(pb), AF.Silu, [("ps", pb)], [("sa", k)])
                    TT("dve", gT[:, fc, :], sa[k][:], bank(pb + 1), ALU.mult, [("sa", k), ("ps", pb + 1)], [("gT", fc)])
                    fcn += 1
            for g in range(4):
                b = go % 2
                go += 1
                DMA("sp", wob[b][:], wo_s[g * 256:(g + 1) * 256, :].rearrange("(f p) x -> p f x", p=128),
                    [(tagw + "o", 2 * g), (tagw + "o", 2 * g + 1)], [("wob", b)], ("wo", 2))
                for f in range(2):
                    dc = 2 * g + f
                    yb = 4 + yn % 2
                    yn += 1
                    for fc in range(NFC):
                        MM(bank(yb), wob[b][:, f, fc * 128:(fc + 1) * 128], gT[:, fc, :], fc == 0, fc == NFC - 1,
                           [("wob", b), ("gT", fc)], [("ps", yb)])
                    STT("dve", xT[:, dc, cols], bank(yb), 0.5, xT[:, dc, cols], ALU.mult, ALU.add, [("ps", yb)], [("xT", t, dc)])
            if post_tile is not None:
                post_tile(t)

    with ExitStack() as st:
        xT = sb("xT1", [128, 8, TOK], F32, st)
        def xload(t):
            for kc in range(8):
                DMA("sp", xT[:, kc, t * 512:(t + 1) * 512], xT_d[:, kc, t * 512:(t + 1) * 512], [], [("xT", t, kc)], ("xin", 8))
            if t == 1:
                cast_rows(w1o_s, w1o_d, 1024, "w1o")
            if t == 2:
                for (d_, s_, n_, tg_) in ((wp_s, wp_d, 14 * 128, "wps"), (wpv_s, wpv_d, 128, "wpvs"), (wmk_s, wmk_d, 256, "wmks"),
                                          (wmv_s, wmv_d, 128, "wmvs"), (bd_s, bd_d, 128, "bds"), (wo_s2, wo_d, 1024, "wos")):
                    cast_rows(d_, s_, n_, tg_)
        xload(0)
        def spill1(t):
            for kc in range(8):
                DMA("sp", x1_s[:, kc, t * 512:(t + 1) * 512], xT[:, kc, t * 512:(t + 1) * 512], [("xT", t, kc)], [("x1_s", t, kc)], ("sp1", 8))
        ffn(xT, w1i_s, w1o_s, 0, "w1", st, prefetch=xload, post_tile=spill1, direct=w1i_d)
        if debug:
            DMA("sp", dbg["d_x1"][:, :, :], xT[:], ALLX, [], ("dbg", 2))
        if stop == 1:
            for kc in range(8):
                DMA("sp", out_d[:, kc, :], xT[:, kc, :], XK(kc), [("out", kc)], ("st", 8))
            R.ops.append(dict(eng="sp", fn=None, r=[("out", kc) for kc in range(8)], w=[], dma=None, inc=0, barrier=False))
        R.barrier()

    def rest():
        X1S = [("x1_s", kc) for kc in range(8)]

        with ExitStack() as st:
            QA = [sb(f"QA{h}", [128, TOK], BF16, st) for h in range(4)]
            KO = [sb(f"KO{h}", [128, TOK], BF16, st) for h in range(4)]
            VO = sb("VO", [128, 16, 260], BF16, st)
            CQ = sb("CQ", [128, 2, TOK], BF16, st)
            GG = sb("GG", [128, 4, TOK], BF16, st)
            KM = sb("KM", [128, 2, NB], F32, st)
            KMH = sb("KMH", [128, 2, NBLK], BF16, st)
            KML = sb("KML", [128, 2, NBLK], BF16, st)
            MK = sb("MK", [128, 2, 256], BF16, st)
            MV = sb("MV", [128, 2, 260], BF16, st)
            pastb = sb("pastb", [128, NB, NBLK], F32, st)
            selp = sb("selp", [128, NB, NBLK], F32, st)
            first = sb("first", [128, 1], F32, st)
            stLX = st
            LX = sb("LX", [128, 4, NB, 259], F32, stLX)
            S1 = sb("S1", [128, 112], F32, stLX)

            def rows(h):
                return slice(0, 64) if h % 2 == 0 else slice(64, 128)

            def vcols(h):
                return slice(65 * h, 65 * h + 128) if h % 2 == 0 else slice(65 * h - 64, 65 * h + 64)

            DMA("sp", pastb[:].rearrange("p a b -> p (a b)"), pastb_d[:, :], [], ["pastb"], LD)
            DMA("sp", selp[:].rearrange("p a b -> p (a b)"), selp_d[:, :], [], ["selp"], LD)
            DMA("sp", first[:], first_d[:, :], [], ["first"], LD)
            MSET("pool", VO[:], 1.0, [("VO", q) for q in range(16)])
            MSET("pool", MV[:], 1.0, ["MV"])
            for h in range(4):
                MSET("pool", QA[h][:], 0.0, [("QA", h)])
                MSET("pool", KO[h][:], 0.0, [("KO", h)])
                ar = slice(96, 100) if h % 2 == 0 else slice(32, 36)
                DMA("sp", QA[h][ar, :], qal_d[h, :, :], [], [("QA", h)], LD)
                DMA("sp", KO[h][ar, :], kal_d[:, :], [], [("KO", h)], LD)

            with ExitStack() as st2:
                WP = sb("WP", [128, 14, 1024], BF16, st2)
                WPV = sb("WPV", [128, 8, 256], BF16, st2)
                sq = sb("psq", [128, 8, 512], BF16, st2)
                rstd = sb("prstd", [128, 512], F32, st2)
                tb = sb("tb0", [128, 512], BF16, st2)
                hr = sb("hr", [128, 512], F32, st2)
                for cc in range(14):
                    DMA("sp", WP[:, cc, :], wp_s[cc * 128:(cc + 1) * 128, :], [("wps", cc)], [("WP", cc)], ("wl", 4))
                DMA("sp", WPV[:].rearrange("p a b -> p (a b)"), wpv_s[:, :], [("wpvs", 0)], ["WPV"], ("wl", 4))

                hcnt = [0]

                def headnorm(ps_ap, n, gc, outs, tag_r, tags_w):
                    hb = 6 + hcnt[0] % 2
                    hcnt[0] += 1
                    ACT(tb[:, :n], ps_ap, AF.Square, [tag_r], ["tb"])
                    MM(PS[:, hb, :n], bones[:], tb[:, :n], True, True, ["tb", "bones"], [("ps", hb)])
                    ACT(hr[:, :n], PS[:, hb, :n], AF.Ln, [("ps", hb)], ["hr"], bias=64.0 * EPS)
                    ACT(hr[:, :n], hr[:, :n], AF.Exp, ["hr"], ["hr"], scale=-0.5)
                    for (dst, rs_), tw in zip(outs, tags_w):
                        STT("dve", dst, ps_ap[rs_, :], gsc[rs_, gc:gc + 1], hr[rs_, :n], ALU.mult, ALU.mult, [tag_r, "hr"], [tw])

                with ExitStack() as stm:
                    WMK = sb("WMK", [128, 2, 1024], BF16, stm)
                    WMV = sb("WMV", [128, 8, 256], BF16, stm)
                    memT = sb("memT", [128, 8, 256], F32, stm)
                    memn = sb("memn", [128, 8, 256], BF16, stm)
                    for cc in range(2):
                        DMA("sp", WMK[:, cc, :], wmk_s[cc * 128:(cc + 1) * 128, :], [("wmks", cc)], ["WMK"], ("wl", 4))
                    DMA("sp", WMV[:].rearrange("p a b -> p (a b)"), wmv_s[:, :], [("wmvs", 0)], ["WMV"], ("wl", 4))
                    DMA("sp", memT[:], memT_d[:, :, :], [], ["memT"], LD)
                    ACT(sq[:, :, :256], memT[:], AF.Square, ["memT"], ["sq"])
                    for kc in range(8):
                        MM(PS[:, 6, :256], ones_b[:], sq[:, kc, :256], kc == 0, kc == 7, ["sq", "ones_b"], [("ps", 6)])
                    ACT(rstd[:, :256], PS[:, 6, :256], AF.Ln, [("ps", 6)], ["rstd"], bias=1024.0 * EPS)
                    ACT(rstd[:, :256], rstd[:, :256], AF.Exp, ["rstd"], ["rstd"], scale=-0.5)
                    for kc in range(8):
                        STT("dve", memn[:, kc, :], memT[:, kc, :], gsc[:, 16 + kc:17 + kc], rstd[:, :256], ALU.mult, ALU.mult,
                            ["memT", "rstd"], [("memn", kc)])
                    for cc in range(2):
                        for kc in range(8):
                            MM(PS[:, cc, :256], WMK[:, cc, kc * 128:(kc + 1) * 128], memn[:, kc, :], kc == 0, kc == 7,
                               ["WMK", ("memn", kc)], [("ps", cc)])
                        headnorm(PS[:, cc, :256], 256, 35, [(MK[0:64, cc, :], slice(0, 64)), (MK[64:128, cc, :], slice(64, 128))],
                                 ("ps", cc), [("MK", cc), ("MK", cc)])
                    for mt in range(2):
                        for kc in range(8):
                            MM(PS[:, 2 + mt, :256], memn[:, kc, mt * 128:(mt + 1) * 128], WMV[:, kc, :], kc == 0, kc == 7,
                               ["WMV", ("memn", kc)], [("ps", 2 + mt)])
                        ACT(MV[:, mt, :].rearrange("p (h e) -> p h e", e=65)[:, :, 0:64],
                            PS[:, 2 + mt, :256].rearrange("p (h e) -> p h e", e=64), AF.Copy, [("ps", 2 + mt)], ["MV"])
                    R.barrier()

                xn = [sb(f"pxn{b}", [128, 8, 512], BF16, st2) for b in range(4)]
                tf = [sb(f"tf{b}", [128, 512], F32, st2) for b in range(4)]
                xtt = sb("xt0", [128, 8, 512], F32, st2)
                LXA = [("LX", c) for c in range(4)]
                uc = [0]
                xnks = []

                def proj(t, cc):
                    cols = slice(t * 512, (t + 1) * 512)
                    xnk = xnks[t]
                    ub = uc[0] % 4
                    uc[0] += 1
                    for kc in range(8):
                        MM(bank(ub), WP[:, cc, kc * 128:(kc + 1) * 128], xn[t][:, kc, :], kc == 0, kc == 7,
                           [("WP", cc), xnk[kc]], [("ps", ub)])
                    pt = ("ps", ub)
                    if cc < 4:
                        ACT(LX[:, cc, 2 * t:2 * t + 2, 3:259], bank(ub).rearrange("p (a b) -> p a b", b=256), AF.Copy, [pt], [("LX", cc)])
                    elif cc < 8:
                        c = cc - 4
                        k = uc[0] % 2
                        g0, g1 = tf[2 * k], tf[2 * k + 1]
                        t0, t1_ = ("tf", 2 * k), ("tf", 2 * k + 1)
                        ACT(g0[:], bank(ub), AF.Copy, [pt], [t0])
                        ACT(g1[:], bank(ub), AF.Square, [pt], [t1_])
                        TS("dve", g1[:], g1[:], 0.044715, 1.0, ALU.mult, ALU.add, [t1_], [t1_])
                        TT("dve", g1[:], g1[:], g0[:], ALU.mult, [t0, t1_], [t1_])
                        ACT(g1[:], g1[:], AF.Sigmoid, [t1_], [t1_], scale=1.5957691216057308)
                        TT("dve", GG[:, c, cols], g0[:], g1[:], ALU.mult, [t0, t1_], [("GG", c)])
                    elif cc < 10:
                        p = cc - 8
                        headnorm(bank(ub), 512, 32, [(QA[2 * p][0:64, cols], slice(0, 64)), (QA[2 * p + 1][64:128, cols], slice(64, 128))],
                                 pt, [("QA", 2 * p), ("QA", 2 * p + 1)])
                    elif cc < 12:
                        p = cc - 10
                        headnorm(bank(ub), 512, 33, [(tf[0][0:64, :], slice(0, 64)), (tf[0][64:128, :], slice(64, 128))],
                                 pt, [("tf", 0), ("tf", 0)])
                        RED("dve", KM[:, p, 2 * t:2 * t + 2], tf[0][:].rearrange("p (a b) -> p a b", b=256), [("tf", 0)], [("KM", p)])
                        ACT(KO[2 * p][0:64, cols], tf[0][0:64, :], AF.Copy, [("tf", 0)], [("KO", 2 * p)])
                        ACT(KO[2 * p + 1][64:128, cols], tf[0][64:128, :], AF.Copy, [("tf", 0)], [("KO", 2 * p + 1)])
                    else:
                        p = cc - 12
                        headnorm(bank(ub), 512, 34, [(CQ[0:64, p, cols], slice(0, 64)), (CQ[64:128, p, cols], slice(64, 128))],
                                 pt, [("CQ", p), ("CQ", p)])

                for t in range(NT):
                    cols = slice(t * 512, (t + 1) * 512)
                    for kc in range(8):
                        DMA("sp", xtt[:, kc, :], x1_s[:, kc, cols], [("x1_s", t, kc)], [("xt", kc)], ("xl", 8))
                    xnks.append(norm_tile(xtt[:], lambda kc: xtt[:, kc, :], 8, xn[t], "xn%d" % t, sq, rstd,
                                          [("xt", kc) for kc in range(8)]))
                    for cc in (10, 11, 0, 1, 2, 3):
                        proj(t, cc)
                TS("dve", KM[:], KM[:], 1.0 / 256.0, None, ALU.mult, None, [("KM", 0), ("KM", 1)], [("KM", 0), ("KM", 1)])
                CP("pool", S1[:, 0:96].rearrange("p (c i k) -> p c i k", c=4, i=NB), LX[:, :, :, 256:259], LXA, ["S1"])
                CP("pool", S1[:, 96:112], KM[:].rearrange("p a b -> p (a b)"), [("KM", 0), ("KM", 1)], ["S1"])
                DMA("pool", s1_in[:, :], S1[:], ["S1"], ["s1_in"], ("x1", 2))
                AG(s1_in, s1_out, ["s1_in"], ["s1_out"], 0)
                for h in range(4):
                    DMA("pool", k_in[h * 64:(h + 1) * 64, :], KO[h][rows(h), :], [("KO", h)], [("k_in", h)], ("x2", 4))
                AG(k_in, k_out, [("k_in", h) for h in range(4)], ["k_out"], 1)
                for t in range(NT):
                    xnk = xnks[t]
                    for s_ in range(4):
                        vb = 4 + s_ % 2
                        for kc in range(8):
                            MM(PS[:, vb, :256], xn[t][:, kc, s_ * 128:(s_ + 1) * 128], WPV[:, kc, :], kc == 0, kc == 7,
                               ["WPV", xnk[kc]], [("ps", vb)])
                        ACT(VO[:, 4 * t + s_, :].rearrange("p (h e) -> p h e", e=65)[:, :, 0:64],
                            PS[:, vb, :256].rearrange("p (h e) -> p h e", e=64), AF.Copy, [("ps", vb)], [("VO", 4 * t + s_)])
                for q in range(16):
                    DMA("pool", v_in[q * 128:(q + 1) * 128, :].rearrange("p (h e) -> p h e", e=64),
                        VO[:, q, :].rearrange("p (h e) -> p h e", e=65)[:, :, 0:64], [("VO", q)], [("v_in", q)], ("x2", 4))
                AG(v_in, v_out, [("v_in", q) for q in range(16)], ["v_out"], 2)
                for t in range(NT):
                    for cc in (8, 9, 12, 13, 4, 5, 6, 7):
                        proj(t, cc)
                R.barrier()
            if stop == 2:
                return

            if stop == 3:
                R.barrier()
                return

            def compute_masks(gm16, t8, thr, mpad):
                for h in range(4):
                    p = h % 2
                    mrow = slice(64, 96) if h % 2 == 0 else slice(0, 32)
                    for qt in range(16):
                        qs = slice(qt * 128, (qt + 1) * 128)
                        MM(PS[:, 7, qt * 32:(qt + 1) * 32], QA[h][rows(h), qs], KMH[rows(h), h // 2, :], True, False, [("QA", h), "KMH"], [("ps", 7)])
                        MM(PS[:, 7, qt * 32:(qt + 1) * 32], QA[h][rows(h), qs], KML[rows(h), h // 2, :], False, True, [("QA", h), "KML"], [("ps", 7)])
                    TT("dve", gm16[:].rearrange("p (i t) n -> p i t n", t=2), PS[:, 7, :].rearrange("p (i t n) -> p i t n", t=2, n=NBLK),
                       pastb[:].unsqueeze(2).broadcast_to([128, NB, 2, NBLK]), ALU.add, [("ps", 7), "pastb"], ["gm16"])
                    for qt in range(16):
                        R.op("dve", (lambda qt: lambda e: e.max(out=t8[:, qt, :], in_=gm16[:, qt, :]))(qt), ["gm16"], [("t8", qt)])
                    TS("dve", thr[:], t8[:, :, 2], -1e8, None, ALU.max, None, [("t8", qt) for qt in range(16)], ["thr"])
                    TT("dve", gm16[:], gm16[:], thr[:].unsqueeze(2).broadcast_to([128, 16, NBLK]), ALU.is_ge, ["gm16", "thr"], ["gm16"])
                    TS("dve", mpad[p][:, :, mrow], gm16[:], -1.0, None, ALU.add, None, ["gm16"], [("mpad", p)])
                    for qt in range(16):
                        MM(PS[:, qt // 4, (qt % 4) * 128:(qt % 4 + 1) * 128], mpad[p][:, qt, :], ident[:], True, True,
                           [("mpad", p), "ident"], [("ps", qt // 4)])
                    ACT(QA[h][mrow, :].rearrange("m (b w) -> m b w", w=512), PS[mrow, 0:4, :], AF.Copy,
                        [("ps", b) for b in range(4)], [("QA", h)], scale=32768.0)


            def run_blocks(blocks, PT, Osbs, rdens, load_k=None, obanks=(6,)):
                NSB = len(PT)
                oc = [0]
                def stageA(n):
                    bk = blocks[n]
                    if bk["pre"] is not None:
                        load_k(bk["pre"])
                    b = n % NSB
                    for s in range(2):
                        MM(bank(2 * b + s), bk["lhs"][s], bk["rhs"], True, bk["extra"] is None, bk["rt"], [("ps", 2 * b + s)])
                        if bk["extra"] is not None:
                            MM(bank(2 * b + s), ident[:], bk["extra"][s], False, True, ["ident", "om"], [("ps", 2 * b + s)])
                    ACT(PT[b][:], bank(2 * b, 2), AF.Exp, [("ps", 2 * b), ("ps", 2 * b + 1)], [("PT", b)])

                def stageB(n):
                    bk = blocks[n]
                    b = n % NSB
                    ob = obanks[oc[0] % len(obanks)]
                    k2 = oc[0] % len(Osbs)
                    Osb, rden = Osbs[k2], rdens[k2]
                    for s in range(2):
                        MM(bank(ob), bk["v"][s], PT[b][:, s, :], bk["first"] and s == 0, bk["last"] and s == 1, [("PT", b)] + bk["rt"], [("ps", ob)])
                    if bk["tail"] is not None:
                        oc[0] += 1
                        h, ydst, tagy = bk["tail"]
                        CP("dve", Osb[:], bank(ob), [("ps", ob)], [("Osb", k2)])
                        selm, tg = (sel_e, "sel_e") if h % 2 == 0 else (sel_o, "sel_o")
                        MM(bank(7), selm[:], Osb[:], True, True, [("Osb", k2), tg], [("ps", 7)])
                        R.op("dve", lambda e: e.reciprocal(out=rden[rows(h), :], in_=PS[rows(h), 7, :]), [("ps", 7)], [("rden", k2)])
                        TT("dve", ydst, Osb[rows(h), :], rden[rows(h), :], ALU.mult, [("Osb", k2), ("rden", k2)],
                           [tagy] + (["attn_started"] if bk.get("mark") else []))

                NBK = len(blocks)
                LEAD = NSB - 1
                for n in range(NBK + LEAD):
                    if n < NBK:
                        stageA(n)
                    if n >= LEAD:
                        stageB(n - LEAD)

            YC = sb("YC", [128, 2, TOK], BF16, st)
            with ExitStack() as st3:
                G1 = sb("G1", [128, G, 112], F32, st3)
                HG = sb("HG", [128, 4, NBLK, 3], F32, st3)
                KMG = sb("KMG", [128, 2, NBLK], F32, st3)
                BD = sb("BD", [128, 2, 4, 128], BF16, st3)
                BT = sb("BT", [128, 4, TOK], F32, st3)
                xb = sb("xb", [128, TOK], F32, st3)
                xbb = sb("xbb", [128, TOK], BF16, st3)
                rr = sb("rr", [128, TOK], F32, st3)
                ii = sb("ii", [128, TOK], F32, st3)
                a2 = sb("a2", [128, TOK], F32, st3)
                hl = sb("hl", [128, TOK], F32, st3)
                tmpS = sb("tmpS", [128, 12, NBLK], F32, st3)
                C2 = sb("C2", [128, 4, NB, 2], F32, st3)
                G2 = sb("G2", [128, G, 64], F32, st3)
                CG = sb("CG", [128, 4, NBLK, 2], F32, st3)
                HS = sb("HS", [128, 4, NBLK], F32, st3)
                hin = sb("hin", [128, 4, NB], F32, st3)
                rs = sb("rs", [128, NB], F32, st3)
                t1 = sb("t1", [128, 4], F32, st3)

                DMA("sp", BD[:].rearrange("p a b c -> p (a b c)"), bd_s[:, :], [("bds", 0)], ["BD"], ("wl", 4))
                DMA("sp", G1[:], s1_out[:, :].rearrange("(r p) x -> p r x", p=128), ["s1_out"], ["G1"], LD)
                for r_ in range(G):
                    for par in range(2):
                        n0 = r_ if par == 0 else 7 - r_
                        CP("pool", HG[:, :, n0::8, :], G1[:, r_, 0:96].rearrange("p (c i k) -> p c i k", c=4, i=NB)[:, :, par::2, :], ["G1"], ["HG"])
                        CP("pool", KMG[:, :, n0::8], G1[:, r_, 96:112].rearrange("p (a i) -> p a i", a=2)[:, :, par::2], ["G1"], ["KMG"])
                CP("dve", KMH[:], KMG[:], ["KMG"], ["KMH"])
                TT("dve", KMG[:], KMG[:], KMH[:], ALU.subtract, ["KMG", "KMH"], ["KMG"])
                CP("dve", KML[:], KMG[:], ["KMG"], ["KML"])
                for i in range(NB):
                    TT("dve", tmpS[:].rearrange("p (c k) n -> p c k n", c=4), HG[:].rearrange("p c n k -> p c k n"),
                       selp[:, i, :].unsqueeze(1).unsqueeze(1).broadcast_to([128, 4, 3, NBLK]), ALU.mult, ["HG", "selp"], ["tmpS"])
                    RED("dve", LX[:, :, i, 0:3], tmpS[:].rearrange("p (c k) n -> p c k n", c=4), ["tmpS"], LXA)
                if debug:
                    DMA("sp", dbg["d_lx"][:, :, :, :], LX[:], LXA, [], ("dbg", 2))

                cwo = sm_offs["cw"][0]
                xb3 = xb[:].rearrange("p (i w) -> p i w", w=256)
                for c in range(4):
                    lx = LX[:, c, :, :]
                    LXc = ("LX", c)

                    def cwc(k, c=c):
                        return small[:, cwo + 4 * c + k:cwo + 4 * c + k + 1]
                    TS("pool", xb3, lx[:, :, 3:259], cwc(3), S("cb")[:, c:c + 1], ALU.mult, ALU.add, [LXc], ["xb"])
                    for k in range(3):
                        STT("dve", xb3, lx[:, :, k:k + 256], cwc(k), xb3, ALU.mult, ALU.add, [LXc, "xb"], ["xb"])
                    ACT(xbb[:], xb[:], AF.Copy, ["xb"], ["xbb"])
                    for q in range(4):
                        MM(bank(q), BD[:, 0, c, :], xbb[:, q * 512:(q + 1) * 512], True, True, ["xbb", "BD"], [("ps", q)])
                        MM(bank(4 + q), BD[:, 1, c, :], xbb[:, q * 512:(q + 1) * 512], True, True, ["xbb", "BD"], [("ps", 4 + q)])
                    ACT(rr[:].rearrange("p (q w) -> p q w", w=512), bank(0, 4), AF.Sigmoid, [("ps", q) for q in range(4)], ["rr"],
                        bias=S("ab")[:, c:c + 1])
                    ACT(ii[:].rearrange("p (q w) -> p q w", w=512), bank(4, 4), AF.Sigmoid, [("ps", 4 + q) for q in range(4)], ["ii"],
                        bias=S("xb")[:, c:c + 1])
                    RED("dve", rs[:], rr[:].rearrange("p (i w) -> p i w", w=256), ["rr"], ["rs"])
                    ACT(C2[:, c, :, 0], rs[:], AF.Exp, ["rs"], [("C2", c)], scale=gsc[:, 36 + c:37 + c])
                    TS("dve", t1[:, c:c + 1], gsc[:, 36 + c:37 + c], 2.0, None, ALU.mult, None, [], [("t1", c)])
                    ACT(a2[:], rr[:], AF.Exp, ["rr", ("t1", c)], ["a2"], scale=t1[:, c:c + 1])
                    ACT(LX[:, c, :, 0:256], rr[:].rearrange("p (i w) -> p i w", w=256), AF.Exp, ["rr", "xb"], [LXc], scale=gsc[:, 36 + c:37 + c])
                    ACT(a2[:], a2[:], AF.Sqrt, ["a2"], ["a2"], scale=-1.0, bias=1.0)
                    TS("dve", rs[:, 0:1], a2[:, 0:1], -1.0, 1.0, ALU.mult, ALU.add, ["a2"], ["rs"])
                    STT("dve", a2[:, 0:1], rs[:, 0:1], first[:, 0:1], a2[:, 0:1], ALU.mult, ALU.add, ["rs", "first", "a2"], ["a2"])
                    TT("pool", ii[:], ii[:], xb[:], ALU.mult, ["ii", "xb"], ["ii"])
                    TT("dve", BT[:, c, :], ii[:], a2[:], ALU.mult, ["ii", "a2"], [("BT", c)])
                    for i in range(NB):
                        SCAN(hl[:, i * 256:(i + 1) * 256], LX[:, c, i, 0:256], BT[:, c, i * 256:(i + 1) * 256], 0.0, [LXc, ("BT", c)], [("hl", i)])
                    CP("dve", C2[:, c, :, 1], hl[:].rearrange("p (i w) -> p i w", w=256)[:, :, 255], [("hl", i) for i in range(NB)], [("C2", c)])
                R.barrier()
                DMA("pool", s2_in[:, :], C2[:].rearrange("p a b c -> p (a b c)"), [("C2", c) for c in range(4)], ["s2_in"], ("x1", 2))
                AG(s2_in, s2_out, ["s2_in"], ["s2_out"], 3)
                Osbm, rdenm = [xb[:, 0:512], xb[:, 512:1024]], [xb[:, 1024:1536], xb[:, 1536:2048]]
                gm16 = rr[:, 0:512].rearrange("p (a n) -> p a n", n=NBLK)
                t8 = rr[:, 512:640].rearrange("p (a k) -> p a k", k=8)
                thr = rr[:, 640:656]
                PTm = [xbb[:, 0:1024].rearrange("p (s w) -> p s w", w=512), xbb[:, 1024:2048].rearrange("p (s w) -> p s w", w=512)]
                hlb = hl[:].bitcast(BF16)
                mpad = [hlb[:, 0:2048].rearrange("p (a m) -> p a m", m=128), hlb[:, 2048:4096].rearrange("p (a m) -> p a m", m=128)]
                MSET("pool", mpad[0], 0.0, [("mpad", 0)])
                MSET("pool", mpad[1], 0.0, [("mpad", 1)])
                mblocks = []
                for h in range(4):
                    p = h // 2
                    for c in range(NT):
                        cols = slice(c * 512, (c + 1) * 512)
                        mblocks.append(dict(lhs=[MK[rows(h), p, 0:128], MK[rows(h), p, 128:256]], rhs=CQ[rows(h), p, cols],
                                            v=[MV[:, 0, vcols(h)], MV[:, 1, vcols(h)]], first=True, last=True,
                                            rt=[("MK", p), ("CQ", p), "MV"], extra=None, tail=(h, YC[rows(h), p, cols], ("YC", p)), pre=None))
                run_blocks(mblocks, PTm, Osbm, rdenm, obanks=(4, 5, 6))
                compute_masks(gm16, t8, thr, mpad)
                R.barrier()
                DMA("sp", G2[:], s2_out[:, :].rearrange("(r p) x -> p r x", p=128), ["s2_out"], ["G2"], LD)
                for r_ in range(G):
                    for par in range(2):
                        n0 = r_ if par == 0 else 7 - r_
                        CP("pool", CG[:, :, n0::8, :], G2[:, r_, :].rearrange("p (c i k) -> p c i k", c=4, i=NB)[:, :, par::2, :], ["G2"], ["CG"])
                for c in range(4):
                    SCAN(HS[:, c, :], CG[:, c, :, 0], CG[:, c, :, 1], 0.0, ["CG"], [("HS", c)])
                for i in range(NB):
                    TT("dve", tmpS[:, 0:4, :], HS[:], selp[:, i, :].unsqueeze(1).broadcast_to([128, 4, NBLK]), ALU.mult,
                       [("HS", c) for c in range(4)] + ["selp"], ["tmpS"])
                    RED("dve", hin[:, :, i], tmpS[:, 0:4, :], ["tmpS"], ["hin"])
                for c in range(4):
                    hb_, hbt = ((hl, "hl"), (rr, "rr"), (ii, "ii"), (a2, "a2"))[c]
                    for i in range(NB):
                        SCAN(hb_[:, i * 256:(i + 1) * 256], LX[:, c, i, 0:256], BT[:, c, i * 256:(i + 1) * 256], hin[:, c, i:i + 1],
                             [("LX", c), ("BT", c), "hin"], [(hbt + "2", i)])
                    TT("pool" if c % 2 == 0 else "dve", GG[:, c, :], hb_[:], GG[:, c, :], ALU.mult,
                       [(hbt + "2", i) for i in range(NB)] + [("GG", c)], [("GG", c)])
                if debug:
                    for c in range(4):
                        CP("dve", xb[:], GG[:, c, :], [("GG", c)], ["xb"])
                        DMA("sp", dbg["d_ylru"][:, c, :], xb[:], ["xb"], ["dbgo"], ("dbg", 2))
                R.barrier()

            YM = sb("YM", [128, 2, TOK], BF16, st)
            if stop == 4:
                return
            with ExitStack() as st4:
                KX = [sb(f"KX{p}", [128, NBLK * 256], BF16, st4) for p in range(2)]
                VA = sb("VA", [128, 64, 260], BF16, st4)
                om = sb("om", [128, 4, 512], BF16, st4)
                NSB = 3
                PT = [sb(f"PT{b}", [128, 2, 512], BF16, st4) for b in range(NSB)]
                Osb = sb("Osb", [128, 512], F32, st4)
                rden = sb("rden", [128, 512], F32, st4)

                R.op("act", lambda e: e.memzero(KX[0][:]), [], [("KX", 0, q) for q in range(8)])
                R.op("act", lambda e: e.memzero(KX[1][:]), [], [("KX", 1, q) for q in range(8)])
                DMA("sp", KX[0][64:100, :], kaugA_d[64:100, :], [], [("KX", 0, q) for q in range(8)], LD)
                DMA("sp", KX[1][0:36, :], kaugB_d[0:36, :], [], [("KX", 1, q) for q in range(8)], LD)
                DMA("sp", om[:], ownmask_d[:, :, :], [], ["om"], LD)
                MSET("pool", VA[:].rearrange("p t (h e) -> p t h e", e=65)[:, :, :, 64:65], 1.0, [("VA", q) for q in range(64)])
                for r_ in range(G):
                    for i in range(NB):
                        n = gblock(r_, i)
                        for s_ in range(2):
                            r0 = r_ * TOK + i * 256 + s_ * 128
                            DMA("sp", VA[:, 2 * n + s_, :].rearrange("p (h e) -> p h e", e=65)[:, :, 0:64],
                                v_out[r0:r0 + 128, :].rearrange("p (h e) -> p h e", e=64), ["v_out"], [("VA", 2 * n + s_)], ("vl", 8))

                def load_k(h):
                    p = h % 2
                    for r_ in range(G):
                        for par in range(2):
                            n0 = r_ if par == 0 else 7 - r_
                            DMA("sp", KX[p][rows(h), :].rearrange("q (c n w) -> q c n w", n=8, w=256)[:, :, n0, :],
                                k_out[r_ * 256 + h * 64:r_ * 256 + (h + 1) * 64, :].rearrange("q (c i w) -> q c i w", i=2, w=256)[:, :, par, :],
                                ["k_out"], [("KX", p, n0)], ("kl", 8))

                blocks = []
                for h in range(4):
                    p = h % 2
                    for c in range(NT):
                        cols = slice(c * 512, (c + 1) * 512)
                        nkb = 8 * c + 7
                        for kb in range(nkb):
                            blocks.append(dict(lhs=[KX[p][:, (2 * kb + s) * 128:(2 * kb + s + 1) * 128] for s in range(2)], rhs=QA[h][:, cols],
                                               v=[VA[:, 2 * kb + s, vcols(h)] for s in range(2)], first=(kb == 0), last=False,
                                               rt=[("KX", p, kb % 8), ("QA", h), ("VA", 2 * kb), ("VA", 2 * kb + 1)], extra=None, tail=None,
                                               pre=(h if (c == 0 and kb == 0) else None)))
                        for kb in range(2):
                            lt = [c * 512 + (2 * kb + s) * 128 for s in range(2)]
                            blocks.append(dict(lhs=[KO[h][:, l0:l0 + 128] for l0 in lt], rhs=QA[h][:, cols],
                                               v=[VO[:, 4 * c + 2 * kb + s, vcols(h)] for s in range(2)], first=False, last=(kb == 1),
                                               rt=[("KO", h), ("QA", h)] + [("VO", 4 * c + 2 * kb + s) for s in range(2)],
                                               extra=[om[:, 2 * kb + s, :] for s in range(2)],
                                               tail=((h, YM[rows(h), h // 2, cols], ("YM", h // 2)) if kb == 1 else None), pre=None,
                                               mark=(h == 0 and c == 0 and kb == 1)))

                run_blocks(blocks, PT, [Osb], [rden], load_k)
                cast_rows(w2i_s, w2i_d, NFC * 128, "w2i", after=["attn_started"])
                cast_rows(w2o_s, w2o_d, 1024, "w2o", after=["attn_started"])
                if debug:
                    dt8 = sb("dt8", [128, TOK], F32, st4)
                    for q in range(2):
                        CP("dve", dt8[:], YM[:, q, :], [("YM", q)], ["dt8"])
                        DMA("sp", dbg["d_ymoba"][:, q, :], dt8[:], ["dt8"], ["dbgo"], ("dbg", 2))
                    for q in range(2):
                        CP("dve", dt8[:], YC[:, q, :], [("YC", q)], ["dt8"])
                        DMA("sp", dbg["d_ymem"][:, q, :], dt8[:], ["dt8"], ["dbgo"], ("dbg", 2))
                    for h in range(4):
                        CP("dve", dt8[:], QA[h][:], [("QA", h)], ["dt8"])
                        DMA("sp", dbg["d_q"][:, h, :], dt8[:], ["dt8"], ["dbgo"], ("dbg", 2))
                    for h in range(4):
                        CP("dve", dt8[:], KO[h][:], [("KO", h)], ["dt8"])
                        DMA("sp", dbg["d_k"][:, h, :], dt8[:], ["dt8"], ["dbgo"], ("dbg", 2))
                R.barrier()

            if stop == 5:
                return
            with ExitStack() as st5:
                WO = sb("WO", [128, 8, 1024], BF16, st5)
                xt = [sb(f"yt{b}", [128, 8, 512], F32, st5) for b in range(2)]
                for dc in range(8):
                    DMA("sp", WO[:, dc, :], wo_s2[dc * 128:(dc + 1) * 128, :], [("wos", dc)], [("WO", dc)], ("wl", 4))
                yn = 0
                for t in range(NT):
                    cols = slice(t * 512, (t + 1) * 512)
                    xtt = xt[t % 2]
                    for kc in range(8):
                        DMA("sp", xtt[:, kc, :], x1_s[:, kc, cols], [("x1_s", t, kc)], [("yt", t % 2, kc)], ("xl", 4))
                    for dc in range(8):
                        yb = yn % 4
                        yn += 1
                        for fc in range(8):
                            if fc < 4:
                                rhs, tg = GG[:, fc, cols], ("GG", fc)
                            elif fc < 6:
                                rhs, tg = YM[:, fc - 4, cols], ("YM", fc - 4)
                            else:
                                rhs, tg = YC[:, fc - 6, cols], ("YC", fc - 6)
                            MM(bank(yb), WO[:, dc, fc * 128:(fc + 1) * 128], rhs, fc == 0, fc == 7, [("WO", dc), tg], [("ps", yb)])
                        TT("dve", xtt[:, dc, :], bank(yb), xtt[:, dc, :], ALU.add, [("ps", yb)], [("yt", t % 2, dc)])
                        DMA("sp", x2_s[:, dc, cols], xtt[:, dc, :], [("yt", t % 2, dc)], [("x2_s", dc)], ("xs", 4))
                R.barrier()

        with ExitStack() as st:
            xT = sb("xT2", [128, 8, TOK], F32, st)
            def x2load(t):
                for kc in range(8):
                    DMA("sp", xT[:, kc, t * 512:(t + 1) * 512], x2_s[:, kc, t * 512:(t + 1) * 512], [("x2_s", kc)], [("xT", t, kc)], ("xin", 8))
            x2load(0)
            if debug:
                DMA("sp", dbg["d_x2"][:, :, :], x2_s[:, :, :], [("x2_s", kc) for kc in range(8)], [], ("dbg", 2))
            ffn(xT, w2i_s, w2o_s, 24, "w2", st, prefetch=x2load)
            for kc in range(8):
                DMA("sp", out_d[:, kc, :], xT[:, kc, :], XK(kc), [("out", kc)], ("st", 8))
            R.ops.append(dict(eng="sp", fn=None, r=[("out", kc) for kc in range(8)], w=[], dma=None, inc=0, barrier=False))
            R.barrier()

    if stop > 1:
        rest()

    chans = R.schedule()
    sems = {c: es.enter_context(nc.semaphore("s_" + "_".join(str(x) for x in (c if isinstance(c, tuple) else (c,))))) for c in chans}
    block = es.enter_context(nc.Block())
    R.emit(nc, block, sems)
    es.close()
    return nc


_CACHE = {}


def kernel(**inp):
    debug = bool(inp.pop("_debug", False))
    inp = {k: np.asarray(v) for k, v in inp.items()}
    wts, offs = _prep_weights(inp)
    ns = wts["small"].shape[1]
    key = (debug,)
    if key not in _CACHE:
        _CACHE[key] = build(offs, ns, debug)
    nc = _CACHE[key]
    tabs = _host_tables()
    x, mem = inp["x"], inp["mem"]
    in_maps = []
    for core in range(8):
        b, j = core // 4, core % 4
        idx = np.concatenate([gblock(j, i) * 256 + np.arange(256) for i in range(NB)])
        xs = x[b][idx]
        xTc = np.ascontiguousarray(xs.T.reshape(8, 128, TOK).transpose(1, 0, 2))
        memT = np.ascontiguousarray(mem[b].T.reshape(8, 128, 256).transpose(1, 0, 2))
        m = dict(xT=xTc, memT=memT)
        m.update(wts)
        m.update(tabs)
        m.update(_core_tables(j))
        in_maps.append(m)
    res = run_bass_kernel_spmd(nc, in_maps, core_ids=list(range(8)))
    out = np.empty((2, 8192, D), np.float32)
    for core in range(8):
        b, j = core // 4, core % 4
        idx = np.concatenate([gblock(j, i) * 256 + np.arange(256) for i in range(NB)])
        oT = res.results[core]["outT"]
        out[b][idx] = oT.transpose(1, 0, 2).reshape(D, TOK).T
    if debug:
        kernel.last = res
    return out
```
